# Optimizing a Trainium2 kernel written in Bass

```python
import math
import jax, jax.numpy as jnp
from jax import lax
import numpy as np

D_MODEL = 1024
BATCH = 4
SEQ = 4096
DEPTH = 4

GRID_W = 64
CTX_LEN = 256

D_HYENA = 256
HYENA_ORDER = 2
HYENA_EMB = 33
HYENA_FILTER_WIDTH = 64
HYENA_TARGET = 1e-2
HYENA_MIN_DECAY = math.log(HYENA_TARGET) / 1.5
HYENA_MAX_DECAY = math.log(HYENA_TARGET) / 0.3
SHORT_CONV = 3
N_DIFF_HEADS = 8
DIFF_QK = 32
DIFF_V = 64
D_DIFF = N_DIFF_HEADS * DIFF_V
D_FNET = 256
FNET_GROUPS = 4
FNET_GROUP_W = D_FNET // FNET_GROUPS
D_MIX = D_HYENA + D_DIFF + D_FNET
ROPE_BASE = 10000.0
ATTN_BLOCK = 128

HY_W = 3 * D_HYENA
QK_W = N_DIFF_HEADS * 2 * DIFF_QK
Q_OFF = HY_W
K_OFF = Q_OFF + QK_W
V_OFF = K_OFF + QK_W
F_OFF = V_OFF + D_DIFF
D_IN = F_OFF + D_FNET

N_EXPERTS = 64
TOP_K = 8
N_GROUPS = 8
TOPK_GROUPS = 4
D_EXPERT = 256
D_SHARED = 256
ROUTED_SCALE = 2.5
MOE_BLOCK = 128

ALPHA = (2 * DEPTH) ** 0.25
BETA = (8 * DEPTH) ** -0.25
LN_EPS = 1e-5

kernel_name = "hybrid_hyena_diffattn_fnet_moe_dit"


def layer_norm(x, g=None, b=None):
    xf = x.astype(jnp.float32)
    mu = jnp.mean(xf, -1, keepdims=True)
    var = jnp.mean(jnp.square(xf - mu), -1, keepdims=True)
    y = (xf - mu) * lax.rsqrt(var + LN_EPS)
    if g is not None:
        y = y * g.astype(jnp.float32) + b.astype(jnp.float32)
    return y.astype(x.dtype)


def rms_norm(x, g):
    xf = x.astype(jnp.float32)
    y = xf * lax.rsqrt(jnp.mean(jnp.square(xf), -1, keepdims=True) + LN_EPS)
    return (y * g.astype(jnp.float32)).astype(x.dtype)


def short_conv(u, w):
    pad = SHORT_CONV // 2
    L = u.shape[1]
    up = jnp.pad(u, ((0, 0), (pad, pad), (0, 0)))
    out = up[:, 0:L] * w[0]
    for j in range(1, SHORT_CONV):
        out = out + up[:, j:j + L] * w[j]
    return out


def hyena_filters(L, w1, b1, w2, b2, w3, freq):
    f32 = jnp.float32
    w1, b1, w2, b2, w3, freq = (a.astype(f32) for a in (w1, b1, w2, b2, w3, freq))
    t = jnp.linspace(0.0, 1.0, L, dtype=f32)[:, None]
    bands = (HYENA_EMB - 1) // 2
    omega = (2.0 * math.pi / L) * jnp.arange(L, dtype=f32)[:, None]
    fb = jnp.linspace(1e-4, bands - 1, bands, dtype=f32)[None, :]
    z = jnp.concatenate([t, jnp.cos(fb * omega), -jnp.sin(fb * omega)], -1)
    a = jnp.sin(freq * (z @ w1 + b1))
    a = jnp.sin(freq * (a @ w2 + b2))
    hc = (a @ w3).reshape(L, HYENA_ORDER, 2, D_HYENA)
    deltas = jnp.abs(jnp.linspace(HYENA_MIN_DECAY, HYENA_MAX_DECAY, D_HYENA, dtype=f32))
    hc = hc * jnp.exp(-t * deltas)[:, None, None, :]
    l1 = jnp.abs(hc[:, :, 0]).sum(0) + jnp.abs(hc[1:, :, 1]).sum(0)
    return hc / l1[None, :, None, :]


def bidir_long_conv(u, h_fwd, h_bwd, bias):
    L = u.shape[1]
    k = jnp.concatenate([h_fwd, jnp.zeros_like(h_fwd[:1]), h_bwd[:0:-1]], 0)
    uf = u.astype(jnp.float32)
    y = jnp.fft.irfft(jnp.fft.rfft(uf, n=2 * L, axis=1) * jnp.fft.rfft(k, axis=0)[None],
                      n=2 * L, axis=1)[:, :L]
    return (y + uf * bias.astype(jnp.float32)).astype(u.dtype)


def hyena_mixer(u, p):
    L = u.shape[1]
    u = short_conv(u, p['short_conv_w'])
    x1, x2, v = jnp.split(u, 3, axis=-1)
    h = hyena_filters(L, p['filt_w1'], p['filt_b1'], p['filt_w2'], p['filt_b2'],
                      p['filt_w3'], p['filt_freq'])
    z = v
    for n, gate in enumerate((x1, x2)):
        z = gate * bidir_long_conv(z, h[:, n, 0], h[:, n, 1], p['hyena_bias'][n])
    return z


def axial_rope_angles(L):
    n = DIFF_QK // 4
    inv = 1.0 / (ROPE_BASE ** (jnp.arange(n, dtype=jnp.float32) / n))
    t = jnp.arange(L)
    row = (t // GRID_W).astype(jnp.float32)
    col = (t % GRID_W).astype(jnp.float32)
    ang = jnp.stack([row[:, None] * inv, col[:, None] * inv], axis=1)
    return jnp.cos(ang), jnp.sin(ang)


def apply_rope(t, cos, sin):
    shp = t.shape
    tr = t.reshape(*shp[:-1], 2, DIFF_QK // 4, 2).astype(jnp.float32)
    a, b = tr[..., 0], tr[..., 1]
    cs, sn = cos[:, None, None], sin[:, None, None]
    out = jnp.stack([a * cs - b * sn, a * sn + b * cs], -1)
    return out.reshape(shp).astype(t.dtype)


def qk_heads(t):
    B, L, _ = t.shape
    return t.reshape(B, L, N_DIFF_HEADS, 2, DIFF_QK)


def to_attn(t):
    return t.transpose(3, 0, 2, 1, 4)


def v_heads(t):
    B, L, _ = t.shape
    return t.reshape(B, L, N_DIFF_HEADS, DIFF_V).transpose(0, 2, 1, 3)


def diff_attn_core(q, k, v, lam):
    s = jnp.einsum('cbhqd,cbhkd->cbhqk', q, k).astype(jnp.float32) * (DIFF_QK ** -0.5)
    a = jax.nn.softmax(s, axis=-1)
    w = a[0] - lam * a[1]
    return jnp.einsum('bhqk,bhkd->bhqd', w.astype(v.dtype), v)


def blocked_diff_attention(q, k, v, lam):
    _, B, H, Lq, DQ = q.shape
    nb = Lq // ATTN_BLOCK
    qb = q.reshape(2, B, H, nb, ATTN_BLOCK, DQ).transpose(3, 0, 1, 2, 4, 5)
    out = lax.map(lambda qq: diff_attn_core(qq, k, v, lam), qb)
    return out.transpose(1, 2, 0, 3, 4).reshape(B, H, Lq, DIFF_V)


def diff_out(o, subln_w, lam_init):
    B, H, L, DV = o.shape
    o = rms_norm(o, subln_w) * (1.0 - lam_init)
    return o.transpose(0, 2, 1, 3).reshape(B, L, H * DV)


def fourier_mixer(u, w_lin):
    B, L, _ = u.shape
    ug = u.astype(jnp.float32).reshape(B, L, FNET_GROUPS, FNET_GROUP_W)
    f = jnp.fft.fftn(ug, axes=(1, 3), norm='ortho').real.astype(u.dtype)
    return jnp.einsum('blgc,gcd->blgd', f, w_lin).reshape(B, L, D_FNET)


def split_proj(z):
    return (z[..., :Q_OFF], z[..., Q_OFF:K_OFF], z[..., K_OFF:V_OFF],
            z[..., V_OFF:F_OFF], z[..., F_OFF:])


def token_mixer(h, hc, p, layer_idx, last):
    L = h.shape[1]
    lam_init = 0.8 - 0.6 * math.exp(-0.3 * layer_idx)
    f32 = jnp.float32
    lam = (jnp.exp(jnp.sum(p['lam_q1'].astype(f32) * p['lam_k1'].astype(f32)))
           - jnp.exp(jnp.sum(p['lam_q2'].astype(f32) * p['lam_k2'].astype(f32))) + lam_init)
    hy, q, k, v, fr = split_proj(h @ p['w_in'])
    cos, sin = axial_rope_angles(L)
    q = to_attn(apply_rope(qk_heads(q), cos, sin))
    k = to_attn(apply_rope(qk_heads(k), cos, sin))
    v = v_heads(v)
    if last:
        kvc = hc @ p['w_in'][:, K_OFF:F_OFF]
        kc, vc = kvc[..., :QK_W], kvc[..., QK_W:]
    else:
        hyc, qc, kc, vc, frc = split_proj(hc @ p['w_in'])
    kc = to_attn(qk_heads(kc))
    vc = v_heads(vc)
    att = blocked_diff_attention(q, jnp.concatenate([k, kc], axis=3),
                                 jnp.concatenate([v, vc], axis=2), lam)
    mixed = jnp.concatenate([hyena_mixer(hy, p), diff_out(att, p['subln_w'], lam_init),
                             fourier_mixer(fr, p['fnet_w'])], -1)
    out = mixed @ p['w_out']
    if last:
        return out, None
    att_c = diff_attn_core(to_attn(qk_heads(qc)), kc, vc, lam)
    mixed_c = jnp.concatenate([hyena_mixer(hyc, p), diff_out(att_c, p['subln_w'], lam_init),
                               fourier_mixer(frc, p['fnet_w'])], -1)
    return out, mixed_c @ p['w_out']


def moe_ffn(h, p):
    T, D = h.shape
    E, G = N_EXPERTS, MOE_BLOCK
    s = jax.nn.sigmoid((h @ p['router_w']).astype(jnp.float32))
    sel = s + p['router_b'].astype(jnp.float32)
    gscore = lax.top_k(sel.reshape(T, N_GROUPS, E // N_GROUPS), 2)[0].sum(-1)
    _, gidx = lax.top_k(gscore, TOPK_GROUPS)
    gmask = jnp.any(gidx[:, :, None] == jnp.arange(N_GROUPS)[None, None, :], axis=1)
    sel = jnp.where(jnp.repeat(gmask, E // N_GROUPS, axis=1), sel, -jnp.inf)
    _, eidx = lax.top_k(sel, TOP_K)
    ws = jnp.take_along_axis(s, eidx, axis=1)
    gates = ws / jnp.sum(ws, -1, keepdims=True) * ROUTED_SCALE
    A = T * TOP_K
    e_flat = eidx.reshape(-1)
    tok_flat = jnp.repeat(jnp.arange(T, dtype=jnp.int32), TOP_K)
    order = jnp.argsort(e_flat)
    e_s, tok_s, g_s = e_flat[order], tok_flat[order], gates.reshape(-1)[order]
    counts = jnp.bincount(e_flat, length=E)
    padded = (counts + G - 1) // G * G
    start = jnp.cumsum(counts) - counts
    cum_pad = jnp.cumsum(padded)
    pstart = cum_pad - padded
    pos = (pstart[e_s] + jnp.arange(A) - start[e_s]).astype(jnp.int32)
    n_blocks = (A + E * (G - 1) + G - 1) // G
    P = n_blocks * G
    rows_tok = jnp.full((P,), T, dtype=jnp.int32).at[pos].set(tok_s)
    gate_pad = jnp.zeros((P,), jnp.float32).at[pos].set(g_s)
    block_exp = jnp.minimum(jnp.searchsorted(cum_pad, jnp.arange(n_blocks) * G, side='right'),
                            E - 1)
    h_pad = jnp.concatenate([h, jnp.zeros((1, D), h.dtype)], 0)

    def expert_block(args):
        r, e = args
        xg = h_pad[r]
        a = jax.nn.silu(xg @ p['exp_w_gate'][e]) * (xg @ p['exp_w_up'][e])
        return a @ p['exp_w_down'][e]

    yb = lax.map(expert_block, (rows_tok.reshape(n_blocks, G), block_exp)).reshape(P, D)
    y = jnp.zeros((T + 1, D), jnp.float32).at[rows_tok].add(
        yb.astype(jnp.float32) * gate_pad[:, None])[:T]
    shared = (jax.nn.silu(h @ p['sh_w_gate']) * (h @ p['sh_w_up'])) @ p['sh_w_down']
    return y.astype(h.dtype) + shared


def trunk_layer(x, xc, c, c_ctx, p, layer_idx, last):
    D = D_MODEL
    B, L, _ = x.shape
    mod = (jax.nn.silu(c) @ p['w_mod'] + p['b_mod'])[:, None, :]
    sh1, sc1, g1, sh2, sc2, g2 = jnp.split(mod, 6, axis=-1)
    n_chunks = 2 if last else 6
    mod_c = jnp.split(jax.nn.silu(c_ctx) @ p['w_mod'][:, :n_chunks * D]
                      + p['b_mod'][:n_chunks * D], n_chunks)
    h = layer_norm(x) * (1.0 + sc1) + sh1
    hc = layer_norm(xc) * (1.0 + mod_c[1]) + mod_c[0]
    mix, mix_c = token_mixer(h, hc, p, layer_idx, last)
    x = layer_norm(ALPHA * x + g1 * mix, p['ln1_g'], p['ln1_b'])
    h = layer_norm(x) * (1.0 + sc2) + sh2
    if last:
        y = moe_ffn(h.reshape(B * L, D), p).reshape(B, L, D)
        return layer_norm(ALPHA * x + g2 * y, p['ln2_g'], p['ln2_b']), None
    cg1, csh2, csc2, cg2 = mod_c[2], mod_c[3], mod_c[4], mod_c[5]
    Lc = xc.shape[1]
    xc = layer_norm(ALPHA * xc + cg1 * mix_c, p['ln1_g'], p['ln1_b'])
    hc = layer_norm(xc) * (1.0 + csc2) + csh2
    y = moe_ffn(jnp.concatenate([h.reshape(B * L, D), hc.reshape(B * Lc, D)], 0), p)
    x = layer_norm(ALPHA * x + g2 * y[:B * L].reshape(B, L, D), p['ln2_g'], p['ln2_b'])
    xc = layer_norm(ALPHA * xc + cg2 * y[B * L:].reshape(B, Lc, D), p['ln2_g'], p['ln2_b'])
    return x, xc


def setup_inputs(seed: int = 0) -> dict:
    key = jax.random.key(seed)
    ks = iter(jax.random.split(key, 64))
    f32 = jnp.float32

    def nrm(shape, scale):
        return jax.random.normal(next(ks), shape, f32) * scale

    Dd, E = D_MODEL, N_EXPERTS
    col_scale = jnp.ones((D_IN,), f32).at[V_OFF:F_OFF].set(BETA)
    return {
        'x': nrm((BATCH, SEQ, Dd), 1.0),
        'c': nrm((BATCH, Dd), 1.0),
        'ctx': nrm((BATCH, CTX_LEN, Dd), 1.0),
        'c_ctx': nrm((Dd,), 1.0),
        'w_mod': nrm((DEPTH, Dd, 6 * Dd), 0.5 * Dd ** -0.5),
        'b_mod': nrm((DEPTH, 6 * Dd), 0.01),
        'w_in': nrm((DEPTH, Dd, D_IN), Dd ** -0.5) * col_scale,
        'w_out': nrm((DEPTH, D_MIX, Dd), BETA * D_MIX ** -0.5),
        'short_conv_w': nrm((DEPTH, SHORT_CONV, HY_W), SHORT_CONV ** -0.5),
        'filt_w1': nrm((DEPTH, HYENA_EMB, HYENA_FILTER_WIDTH), HYENA_EMB ** -0.5),
        'filt_b1': nrm((DEPTH, HYENA_FILTER_WIDTH), 0.01),
        'filt_w2': nrm((DEPTH, HYENA_FILTER_WIDTH, HYENA_FILTER_WIDTH), HYENA_FILTER_WIDTH ** -0.5),
        'filt_b2': nrm((DEPTH, HYENA_FILTER_WIDTH), 0.01),
        'filt_w3': nrm((DEPTH, HYENA_FILTER_WIDTH, HYENA_ORDER * 2 * D_HYENA), HYENA_FILTER_WIDTH ** -0.5),
        'filt_freq': 1.0 + nrm((DEPTH, HYENA_FILTER_WIDTH), 0.01),
        'hyena_bias': nrm((DEPTH, HYENA_ORDER, D_HYENA), 0.1),
        'lam_q1': nrm((DEPTH, DIFF_QK), 0.1),
        'lam_k1': nrm((DEPTH, DIFF_QK), 0.1),
        'lam_q2': nrm((DEPTH, DIFF_QK), 0.1),
        'lam_k2': nrm((DEPTH, DIFF_QK), 0.1),
        'subln_w': 1.0 + nrm((DEPTH, DIFF_V), 0.01),
        'fnet_w': nrm((DEPTH, FNET_GROUPS, FNET_GROUP_W, FNET_GROUP_W), FNET_GROUP_W ** -0.5),
        'ln1_g': 1.0 + nrm((DEPTH, Dd), 0.01),
        'ln1_b': nrm((DEPTH, Dd), 0.01),
        'ln2_g': 1.0 + nrm((DEPTH, Dd), 0.01),
        'ln2_b': nrm((DEPTH, Dd), 0.01),
        'router_w': nrm((DEPTH, Dd, E), Dd ** -0.5),
        'router_b': nrm((DEPTH, E), 0.01),
        'exp_w_gate': nrm((DEPTH, E, Dd, D_EXPERT), Dd ** -0.5),
        'exp_w_up': nrm((DEPTH, E, Dd, D_EXPERT), Dd ** -0.5),
        'exp_w_down': nrm((DEPTH, E, D_EXPERT, Dd), BETA * D_EXPERT ** -0.5),
        'sh_w_gate': nrm((DEPTH, Dd, D_SHARED), Dd ** -0.5),
        'sh_w_up': nrm((DEPTH, Dd, D_SHARED), Dd ** -0.5),
        'sh_w_down': nrm((DEPTH, D_SHARED, Dd), BETA * D_SHARED ** -0.5),
    }


def reference(x, c, ctx, c_ctx, w_mod, b_mod, w_in, w_out, short_conv_w, filt_w1, filt_b1,
              filt_w2, filt_b2, filt_w3, filt_freq, hyena_bias, lam_q1, lam_k1, lam_q2, lam_k2,
              subln_w, fnet_w, ln1_g, ln1_b, ln2_g, ln2_b, router_w, router_b, exp_w_gate,
              exp_w_up, exp_w_down, sh_w_gate, sh_w_up, sh_w_down):
    xc = ctx
    for i in range(DEPTH):
        p = {
            'w_mod': w_mod[i], 'b_mod': b_mod[i], 'w_in': w_in[i], 'w_out': w_out[i],
            'short_conv_w': short_conv_w[i], 'filt_w1': filt_w1[i], 'filt_b1': filt_b1[i],
            'filt_w2': filt_w2[i], 'filt_b2': filt_b2[i], 'filt_w3': filt_w3[i],
            'filt_freq': filt_freq[i], 'hyena_bias': hyena_bias[i],
            'lam_q1': lam_q1[i], 'lam_k1': lam_k1[i], 'lam_q2': lam_q2[i], 'lam_k2': lam_k2[i],
            'subln_w': subln_w[i], 'fnet_w': fnet_w[i],
            'ln1_g': ln1_g[i], 'ln1_b': ln1_b[i], 'ln2_g': ln2_g[i], 'ln2_b': ln2_b[i],
            'router_w': router_w[i], 'router_b': router_b[i],
            'exp_w_gate': exp_w_gate[i], 'exp_w_up': exp_w_up[i], 'exp_w_down': exp_w_down[i],
            'sh_w_gate': sh_w_gate[i], 'sh_w_up': sh_w_up[i], 'sh_w_down': sh_w_down[i],
        }
        x, xc = trunk_layer(x, xc, c, c_ctx, p, i, i == DEPTH - 1)
    return x
```

```python
import math
import ml_dtypes
import numpy as np
import concourse.bass as bass
import concourse.mybir as mybir
from concourse.bass_utils import run_bass_kernel_spmd

F32 = mybir.dt.float32
BF16 = mybir.dt.bfloat16
I32 = mybir.dt.int32
U32 = mybir.dt.uint32
AF = mybir.ActivationFunctionType
ALU = mybir.AluOpType
AX = mybir.AxisListType

ENGS = ["tensor", "vector", "scalar", "gpsimd", "sync"]
NDMA = 6


class Sched:
    def __init__(self, nc):
        self.nc = nc
        self.ops = {e: [] for e in ENGS}
        self.cnt = {e: 0 for e in ENGS}
        self.done = {e: nc.alloc_semaphore(name=f"done_{e}") for e in ENGS}
        self.dsem = {q: [nc.alloc_semaphore(name=f"dma_{q}_{i}") for i in range(NDMA)]
                     for q in ("sync", "gpsimd", "scalar")}
        self.dtot = {q: [0] * NDMA for q in self.dsem}
        self.dn = {q: 0 for q in self.dsem}
        self.lastw = {}
        self.readers = {}
        self.waited = {e: {} for e in ENGS}
        self.out_tokens = []
        self.n_instr = 0
        self.ccs = []
        self.bg_ccs = set()
        self.persist = {}

    def _deps(self, eng, reads, writes):
        toks = []
        for k in reads:
            t = self.lastw.get(k)
            if t is None:
                t = self.persist.get(k)
            if t is not None:
                toks.append(t)
        for k in writes:
            t = self.lastw.get(k)
            if t is not None:
                toks.append(t)
            toks.extend(self.readers.get(k, ()))
        need = {}
        for (s, v, own) in toks:
            if own == eng and eng == "tensor":
                continue
            if v > need.get(id(s), (None, 0))[1]:
                need[id(s)] = (s, v)
        out = []
        w = self.waited[eng]
        for sid, (s, v) in need.items():
            if w.get(sid, 0) >= v:
                continue
            w[sid] = v
            out.append((s, v))
        return out

    def _commit(self, tok, reads, writes):
        for k in writes:
            self.lastw[k] = tok
            self.readers[k] = []
        for k in reads:
            self.readers.setdefault(k, []).append(tok)

    def op(self, eng, fns, reads=(), writes=()):
        if callable(fns):
            fns = [fns]
        waits = self._deps(eng, reads, writes)
        self.cnt[eng] += 1
        tok = (self.done[eng], self.cnt[eng], eng)
        self.ops[eng].append((waits, fns, (self.done[eng], 1)))
        self._commit(tok, reads, writes)
        self.n_instr += len(fns)
        return tok

    def dma(self, q, fn, reads=(), writes=(), is_output=False):
        waits = self._deps(q, reads, writes)
        i = self.dn[q] % NDMA
        self.dn[q] += 1
        s = self.dsem[q][i]
        prev = self.dtot[q][i]
        w = self.waited[q]
        if prev > 0 and w.get(id(s), 0) < prev:
            waits.append((s, prev))
            w[id(s)] = prev
        self.dtot[q][i] = prev + 16
        tok = (s, prev + 16, "dma_" + q)
        self.ops[q].append((waits, [fn], (s, 16)))
        self._commit(tok, reads, writes)
        if is_output:
            self.out_tokens.append(tok)
        self.n_instr += 1
        return tok

    def collective(self, fn, reads=(), writes=(), background=False):
        eng = "gpsimd"
        waits = self._deps(eng, reads, writes)
        sem = self.nc.alloc_semaphore(name=f"cc{len(self.ccs)}")
        self.ccs.append(sem)
        tok = (sem, 1, "cc")
        self.ops[eng].append((waits, [fn], (sem, None)))
        self._commit(tok, reads, writes)
        if background:
            self.bg_ccs.add(id(sem))
            for k in writes:
                self.persist[k] = tok
        self.n_instr += 1
        return tok

    def barrier(self):
        targets = [(sm, 1) for sm in self.ccs if id(sm) not in self.bg_ccs]
        for e in ENGS:
            if self.cnt[e] > 0:
                targets.append((self.done[e], self.cnt[e]))
        for q in self.dsem:
            for i, sm in enumerate(self.dsem[q]):
                if self.dtot[q][i] > 0:
                    targets.append((sm, self.dtot[q][i]))
        for e in ENGS:
            waits = []
            w = self.waited[e]
            for (sm, v) in targets:
                if w.get(id(sm), 0) < v:
                    waits.append((sm, v))
                    w[id(sm)] = v
            self.cnt[e] += 1
            self.ops[e].append((waits, [lambda eng: eng.nop()], (self.done[e], 1)))
        self.lastw.clear()
        self.readers.clear()

    def new_epoch(self):
        self.epoch = getattr(self, "epoch", 0) + 1
        for e in ENGS:
            self.done[e] = self.nc.alloc_semaphore(name=f"done_{e}_{self.epoch}")
            self.cnt[e] = 0

    def finish(self):
        nc = self.nc
        final = {}
        for (s, v, _) in self.out_tokens:
            if v > final.get(id(s), (None, 0))[1]:
                final[id(s)] = (s, v)
        for q in self.dsem:
            for i, s in enumerate(self.dsem[q]):
                if self.dtot[q][i] > 0:
                    final[id(s)] = (s, self.dtot[q][i])
        for e in ENGS:
            if e != "sync" and self.cnt[e] > 0:
                final[id(self.done[e])] = (self.done[e], self.cnt[e])
        for sm in self.ccs:
            final[id(sm)] = (sm, 1)
        ops = self.ops
        with nc.Block() as block:
            def mk(ename):
                def body(eng):
                    for (waits, fns, (sem, inc)) in ops[ename]:
                        for (s, v) in waits:
                            eng.wait_ge(s, v)
                        for f in fns[:-1]:
                            f(eng)
                        if inc is None:
                            fns[-1](eng).then_inc(sem)
                        else:
                            fns[-1](eng).then_inc(sem, inc)
                    if ename == "sync":
                        for (s, v) in final.values():
                            eng.wait_ge(s, v)
                return body
            block.tensor(mk("tensor"))
            block.vector(mk("vector"))
            block.scalar(mk("scalar"))
            block.gpsimd(mk("gpsimd"))
            block.sync(mk("sync"))


NPS = 7


class Ctx:
    def __init__(self):
        self.nc = bass.Bass("TRN2", target_bir_lowering=False)
        self.S = Sched(self.nc)
        self.ps = [self.nc.alloc_psum_tensor(f"ps{i}", [128, 512], F32).ap() for i in range(7)]
        self.psb = self.nc.alloc_psum_tensor("psb", [128, 1024], BF16).ap()
        self.psi = 0
        self.rot = {}
        self.override = {}
        self.prefix = ""
        self.declared = {}
        self.guards = []
        self.uid = 0

    def _dram(self, name, shape, dt, kind):
        if name in self.override:
            return self.override[name]
        full = self.prefix + name
        if full not in self.declared:
            self.declared[full] = self.nc.dram_tensor(full, list(shape), dt, kind=kind).ap()
        return self.declared[full]

    def din(self, name, shape, dt=F32):
        return self._dram(name, shape, dt, "ExternalInput")

    def dout(self, name, shape, dt=F32):
        return self._dram(name, shape, dt, "ExternalOutput")

    def scratch(self, name, shape, dt=F32):
        return self.nc.dram_tensor(name, list(shape), dt).ap()

    def sb(self, name, shape, dt=F32):
        self.uid += 1
        g = self.nc.sbuf_tensor(f"{name}_{self.uid}", list(shape), dt)
        h = g.__enter__()
        self.guards.append(g)
        return h.ap()

    def mark(self):
        return len(self.guards)

    def release(self, mark):
        self.S.barrier()
        while len(self.guards) > mark:
            self.guards.pop().__exit__(None, None, None)
        self.rot = {}

    def psum(self):
        i = self.psi % NPS
        self.psi += 1
        return self.ps[i], ("ps", i)

    def rotbuf(self, name, shape, dt, n):
        if name not in self.rot:
            self.rot[name] = [[self.sb(f"{name}{i}", shape, dt) for i in range(n)], 0]
        bufs, i = self.rot[name]
        self.rot[name][1] = i + 1
        return bufs[i % n], (name, i % n)

    def load(self, q, dst, src, key, reads=()):
        return self.S.dma(q, lambda e: e.dma_start(out=dst, in_=src), reads=list(reads), writes=[key])

    def store(self, q, dst, src, key):
        return self.S.dma(q, lambda e: e.dma_start(out=dst, in_=src), reads=[key], is_output=True)


def run_prog(c, in_maps):
    c.S.finish()
    res = run_bass_kernel_spmd(c.nc, in_maps, core_ids=list(range(8)))
    return res.results


def build_M():
    c = Ctx()
    S = c.S
    ccT = c.din("ccT", [128, 8, 5])
    wm = c.din("wm", [1024, 768])
    bm = c.din("bm", [5, 768])
    out = c.dout("mod", [5, 768])
    ccs = c.sb("ccs", [128, 8, 5])
    scs = c.sb("scs", [128, 8, 5])
    wsb = c.sb("wsb", [128, 8, 768])
    bmb = c.sb("bmb", [5, 768])
    res = c.sb("res", [5, 768])
    c.load("sync", ccs, ccT, "ccs")
    c.load("sync", wsb, wm.rearrange("(k p) n -> p k n", p=128), "wsb")
    c.load("sync", bmb, bm, "bmb")
    S.op("scalar", lambda e: e.activation(out=scs, in_=ccs, func=AF.Silu), reads=["ccs"], writes=["scs"])
    for h in range(2):
        ps, pk = c.psum()
        S.op("tensor", [(lambda e, kc=kc, ps=ps, h=h: e.matmul(ps[0:5, 0:384], lhsT=scs[:, kc, :], rhs=wsb[:, kc, h * 384:(h + 1) * 384],
                                                   start=(kc == 0), stop=(kc == 7))) for kc in range(8)],
             reads=["scs", "wsb"], writes=[pk])
        S.op("vector", lambda e, ps=ps, h=h: e.tensor_tensor(out=res[:, h * 384:(h + 1) * 384], in0=ps[0:5, 0:384],
                                                 in1=bmb[:, h * 384:(h + 1) * 384], op=ALU.add),
             reads=[pk, "bmb"], writes=["res"])
    c.store("sync", out, res, "res")
    return c


def silu_layout_cc(cvec, c_ctx):
    cc = np.concatenate([cvec, c_ctx[None]], 0).astype(np.float32)
    return np.ascontiguousarray(cc.T.reshape(8, 128, 5).transpose(1, 0, 2))


def run_M(prog, cvec, c_ctx, w_mod_l, b_mod_l):
    ccT = silu_layout_cc(cvec, c_ctx)
    maps = []
    for r in range(8):
        maps.append({"ccT": ccT, "wm": np.ascontiguousarray(w_mod_l[:, r * 768:(r + 1) * 768]),
                     "bm": np.ascontiguousarray(np.broadcast_to(b_mod_l[None, r * 768:(r + 1) * 768], (5, 768)))})
    res = run_bass_kernel_spmd(prog.nc, maps, core_ids=list(range(8))).results
    return np.concatenate([res[r]["mod"] for r in range(8)], 1)


LN_EPS = 1e-5
ALPHA = 8.0 ** 0.25
TPC = 17


def ln_tile(c, src, skey, dst, dkey, epsb):
    S = c.S
    st, stk = c.rotbuf("ln_st", [128, 12], F32, 3)
    mv, mvk = c.rotbuf("ln_mv", [128, 2], F32, 3)
    sd, sdk = c.rotbuf("ln_sd", [128, 1], F32, 3)
    rs, rsk = c.rotbuf("ln_rs", [128, 1], F32, 3)
    S.op("vector", [lambda e: e.bn_stats(out=st[:, 0:6], in_=src[:, 0:512]),
                    lambda e: e.bn_stats(out=st[:, 6:12], in_=src[:, 512:1024])], reads=[skey], writes=[stk])
    S.op("vector", lambda e: e.bn_aggr(out=mv, in_=st), reads=[stk], writes=[mvk])
    S.op("scalar", lambda e: e.activation(out=sd, in_=mv[:, 1:2], func=AF.Sqrt, bias=epsb[:, 0:1], scale=1.0),
         reads=[mvk, "epsb"], writes=[sdk])
    S.op("vector", lambda e: e.reciprocal(out=rs, in_=sd), reads=[sdk], writes=[rsk])
    S.op("vector", lambda e: e.tensor_scalar(out=dst, in0=src, scalar1=mv[:, 0:1], scalar2=rs[:, 0:1],
                                             op0=ALU.subtract, op1=ALU.mult), reads=[skey, mvk, rsk], writes=[dkey])


def make_eps(c):
    epsb = c.sb("epsb", [128, 1], F32)
    c.S.op("vector", lambda e: e.memset(epsb, LN_EPS), writes=["epsb"])
    return epsb


def build_A1a():
    c = Ctx()
    S = c.S
    x = c.din("x", [TPC * 128, 1024])
    scd = c.din("sc", [128, 2, 1024])
    shd = c.din("sh", [128, 2, 1024])
    h = c.dout("h", [TPC * 128, 1024], BF16)
    sc = c.sb("scs", [128, 2, 1024])
    sh = c.sb("shs", [128, 2, 1024])
    epsb = make_eps(c)
    c.load("sync", sc, scd, "sc")
    c.load("sync", sh, shd, "sh")
    S.op("vector", lambda e: e.tensor_scalar(out=sc, in0=sc, scalar1=1.0, scalar2=None, op0=ALU.add), reads=["sc"], writes=["sc"])
    for t in range(TPC):
        m = 1 if t == TPC - 1 else 0
        xt, xk = c.rotbuf("xt", [128, 1024], F32, 2)
        xn, nk = c.rotbuf("xn", [128, 1024], F32, 2)
        t2, tk = c.rotbuf("t2", [128, 1024], F32, 2)
        ho, hk = c.rotbuf("ho", [128, 1024], BF16, 2)
        c.load("sync", xt, x[t * 128:(t + 1) * 128, :], xk)
        ln_tile(c, xt, xk, xn, nk, epsb)
        S.op("vector", lambda e, xn=xn, t2=t2, m=m: e.tensor_tensor(out=t2, in0=xn, in1=sc[:, m, :], op=ALU.mult), reads=[nk, "sc"], writes=[tk])
        S.op("gpsimd", lambda e, t2=t2, ho=ho, m=m: e.tensor_tensor(out=ho, in0=t2, in1=sh[:, m, :], op=ALU.add), reads=[tk, "sh"], writes=[hk])
        c.store("sync", h[t * 128:(t + 1) * 128, :], ho, hk)
    return c


def bc128(v):
    return np.ascontiguousarray(np.broadcast_to(np.asarray(v, np.float32)[None], (128,) + tuple(np.shape(v))))


def build_A1b(c=None, hTs=None):
    c = c or Ctx()
    S = c.S
    if hTs is None:
        hT = c.din("hT", [1024, 4356], BF16)
    w = c.din("w", [1024, 1664])
    scw = c.din("scw", [128, 3, 384])
    wfT = c.din("wfT", [64, 2, 1024])
    fw = c.din("fw", [64, 2, 64])
    c64 = c.din("c64", [64, 2, 64])
    ropec = c.din("ropec", [128, 4352])
    ropes = c.din("ropes", [128, 4352])
    hyX = c.dout("hyX", [64, 384, 64], BF16)
    pX = c.dout("pX", [64, 256, 64], BF16)
    hyc = c.dout("hyc", [256, 384], BF16)
    pc = c.dout("pc", [256, 256], BF16)
    qT = c.dout("qT", [256, 4352], BF16)
    kT = c.dout("kT", [256, 4352], BF16)
    V = c.dout("V", [4352, 256], BF16)
    if hTs is None:
        hTs = c.sb("hTs", [128, 8, 4356], BF16)
        for kc in range(8):
            c.load("sync", hTs[:, kc, :], hT[kc * 128:(kc + 1) * 128, :], "hT")
    Wb = c.sb("Wb", [128, 8, 2688], BF16)
    mprep = c.mark()
    Whf = c.sb("Whf", [128, 8, 384], F32)
    scs = c.sb("scws", [128, 3, 384], F32)
    wfTs = c.sb("wfTs", [64, 2, 1024], F32)
    fws = c.sb("fws", [64, 2, 64], F32)
    c64s = c.sb("c64s", [64, 2, 64], F32)
    Ms = c.sb("Ms", [64, 4, 64], F32)
    wv = w.rearrange("(k p) n -> p k n", p=128)
    for kc in range(8):
        S.dma("gpsimd", lambda e, kc=kc: e.dma_start(out=Wb[:, kc, 1152:2432], in_=wv[:, kc, 384:1664]), writes=["Wb"])
    c.load("sync", Whf, wv[:, :, 0:384], "Whf")
    c.load("sync", scs, scw, "scs")
    c.load("sync", wfTs, wfT, "wfTs")
    c.load("sync", fws, fw, "fws")
    c.load("sync", c64s, c64, "c64s")
    for j in range(3):
        S.op("vector", [(lambda e, j=j, kc=kc: e.tensor_tensor(out=Wb[:, kc, j * 384:(j + 1) * 384], in0=Whf[:, kc, :],
                                                               in1=scs[:, j, :], op=ALU.mult)) for kc in range(8)],
             reads=["Whf", "scs"], writes=["Wb"])
    ps, pk = c.psum()
    S.op("tensor", [(lambda e, g=g, t=t, ps=ps: e.matmul(ps[0:64, (g * 2 + t) * 64:(g * 2 + t + 1) * 64], lhsT=c64s[:, t, :],
                                                        rhs=fws[:, g, :], start=True, stop=True)) for g in range(2) for t in range(2)],
         reads=["c64s", "fws"], writes=[pk])
    S.op("vector", lambda e, ps=ps: e.tensor_copy(out=Ms.rearrange("p a b -> p (a b)"), in_=ps[0:64, 0:256]), reads=[pk], writes=["Ms"])
    for kc in range(8):
        ps, pk = c.psum()
        S.op("tensor", [(lambda e, g=g, t=t, ps=ps, kc=kc: e.matmul(ps[:, (t * 2 + g) * 64:(t * 2 + g + 1) * 64],
                                                                    lhsT=wfTs[:, g, kc * 128:(kc + 1) * 128], rhs=Ms[:, g * 2 + t, :],
                                                                    start=True, stop=True)) for t in range(2) for g in range(2)],
             reads=["wfTs", "Ms"], writes=[pk])
        S.op("vector", lambda e, ps=ps, kc=kc: e.tensor_copy(out=Wb[:, kc, 2432:2688], in_=ps[:, 0:256]), reads=[pk], writes=["Wb"])
    c.release(mprep)
    XS = c.sb("XS", [64, 384, 64], BF16)

    def mmgroup(ps_ap, pairs, pk):
        n = len(pairs)
        S.op("tensor", [(lambda e, i=i, l=l, r=r: e.matmul(ps_ap, lhsT=l, rhs=r, start=(i == 0), stop=(i == n - 1)))
                        for i, (l, r) in enumerate(pairs)], reads=["hT", "Wb"], writes=[pk])

    chunks = [(1 + 512 * i, 512 * i, 512) for i in range(8)] + [(4099, 4096, 256)]
    for (hc0, t0, n) in chunks:
        rcs, rck = c.rotbuf("ropc", [128, 512], F32, 2)
        rss, rsk = c.rotbuf("rops", [128, 512], F32, 2)
        c.load("sync", rcs[:, 0:n], ropec[:, t0:t0 + n], rck)
        c.load("sync", rss[:, 0:n], ropes[:, t0:t0 + n], rsk)
        for (c0, c0sw, outT) in ((1152, 1408, qT), (1664, 1920, kT)):
            for rc in range(2):
                psA, pkA = c.psum()
                mmgroup(psA[:, 0:n], [(Wb[:, kc, c0 + rc * 128:c0 + (rc + 1) * 128], hTs[:, kc, hc0:hc0 + n]) for kc in range(8)], pkA)
                psB, pkB = c.psum()
                mmgroup(psB[:, 0:n], [(Wb[:, kc, c0sw + rc * 128:c0sw + (rc + 1) * 128], hTs[:, kc, hc0:hc0 + n]) for kc in range(8)], pkB)
                t1, t1k = c.rotbuf("rt1", [128, 512], F32, 2)
                t2, t2k = c.rotbuf("rt2", [128, 512], F32, 2)
                qo, qok = c.rotbuf("rqo", [128, 512], BF16, 3)
                S.op("vector", lambda e, t1=t1, psA=psA, rcs=rcs, n=n: e.tensor_tensor(out=t1[:, 0:n], in0=psA[:, 0:n], in1=rcs[:, 0:n], op=ALU.mult),
                     reads=[pkA, rck], writes=[t1k])
                S.op("vector", lambda e, t2=t2, psB=psB, rss=rss, n=n: e.tensor_tensor(out=t2[:, 0:n], in0=psB[:, 0:n], in1=rss[:, 0:n], op=ALU.mult),
                     reads=[pkB, rsk], writes=[t2k])
                S.op("gpsimd", lambda e, t1=t1, t2=t2, qo=qo, n=n: e.tensor_tensor(out=qo[:, 0:n], in0=t1[:, 0:n], in1=t2[:, 0:n], op=ALU.add),
                     reads=[t1k, t2k], writes=[qok])
                c.store("sync", outT[rc * 128:(rc + 1) * 128, t0:t0 + n], qo[:, 0:n], qok)
    for tt in range(34):
        hc0 = 1 + 128 * tt if tt < 32 else 4099 + 128 * (tt - 32)
        ps, pk = c.psum()
        mmgroup(ps[:, 0:256], [(hTs[:, kc, hc0:hc0 + 128], Wb[:, kc, 2176:2432]) for kc in range(8)], pk)
        vo, vok = c.rotbuf("vo", [128, 256], BF16, 3)
        S.op("scalar", lambda e, vo=vo, ps=ps: e.copy(out=vo, in_=ps[:, 0:256]), reads=[pk], writes=[vok])
        c.store("sync", V[tt * 128:(tt + 1) * 128, :], vo, vok)
    for n2 in range(64):
        ps, pk = c.psum()
        mmgroup(ps[0:64, 0:384], [(hTs[:, kc, n2 + j:n2 + j + 4096:64], Wb[:, kc, j * 384:(j + 1) * 384]) for j in range(3) for kc in range(8)], pk)
        if n2 % 2:
            S.op("scalar", lambda e, ps=ps, n2=n2: e.copy(out=XS[:, :, n2], in_=ps[0:64, 0:384]), reads=[pk], writes=["XS"])
        else:
            S.op("vector", lambda e, ps=ps, n2=n2: e.tensor_copy(out=XS[:, :, n2], in_=ps[0:64, 0:384]), reads=[pk], writes=["XS"])
    c.store("sync", hyX, XS, "XS")
    for n2 in range(64):
        ps, pk = c.psum()
        mmgroup(ps[0:64, 0:256], [(hTs[:, kc, 1 + n2:1 + n2 + 4096:64], Wb[:, kc, 2432:2688]) for kc in range(8)], pk)
        if n2 % 2:
            S.op("vector", lambda e, ps=ps, n2=n2: e.tensor_copy(out=XS[:, 0:256, n2], in_=ps[0:64, 0:256]), reads=[pk], writes=["XS"])
        else:
            S.op("scalar", lambda e, ps=ps, n2=n2: e.copy(out=XS[:, 0:256, n2], in_=ps[0:64, 0:256]), reads=[pk], writes=["XS"])
    c.store("sync", pX, XS[:, 0:256, :], "XS")
    for j2 in range(2):
        ps, pk = c.psum()
        mmgroup(ps[:, 0:384], [(hTs[:, kc, 4098 + j + 128 * j2:4098 + j + 128 * j2 + 128], Wb[:, kc, j * 384:(j + 1) * 384]) for j in range(3) for kc in range(8)], pk)
        ho, hok = c.rotbuf("hco", [128, 384], BF16, 2)
        S.op("vector", lambda e, ho=ho, ps=ps: e.tensor_copy(out=ho, in_=ps[:, 0:384]), reads=[pk], writes=[hok])
        c.store("sync", hyc[j2 * 128:(j2 + 1) * 128, :], ho, hok)
        ps, pk = c.psum()
        mmgroup(ps[:, 0:256], [(hTs[:, kc, 4099 + 128 * j2:4099 + 128 * j2 + 128], Wb[:, kc, 2432:2688]) for kc in range(8)], pk)
        po, pok = c.rotbuf("pco", [128, 256], BF16, 2)
        S.op("vector", lambda e, po=po, ps=ps: e.tensor_copy(out=po, in_=ps[:, 0:256]), reads=[pk], writes=[pok])
        c.store("sync", pc[j2 * 128:(j2 + 1) * 128, :], po, pok)
    return c


D_HY, QK_W, Q_OFF, K_OFF, V_OFF, F_OFF = 256, 512, 768, 1280, 1792, 2304
L_MAIN, L_CTX, GRID_W = 4096, 256, 64


def rope_tables():
    n = 8
    inv = (1.0 / (10000.0 ** (np.arange(n, dtype=np.float32) / n))).astype(np.float32)
    t = np.arange(L_MAIN)
    pos = np.stack([(t // GRID_W).astype(np.float32), (t % GRID_W).astype(np.float32)], 0)
    cosT = np.ones((32, L_MAIN + L_CTX), np.float32)
    sinT = np.zeros((32, L_MAIN + L_CTX), np.float32)
    for d in range(32):
        ax, pr, par = d // 16, (d % 16) // 2, d % 2
        ang = (pos[ax] * inv[pr]).astype(np.float32)
        cosT[d, :L_MAIN] = np.cos(ang)
        sinT[d, :L_MAIN] = np.sin(ang) * (1.0 if par else -1.0)
    return np.ascontiguousarray(np.tile(cosT, (4, 1))), np.ascontiguousarray(np.tile(sinT, (4, 1)))


def dft64_consts():
    j = np.arange(64)
    ang = 2 * np.pi * np.outer(j, j) / 64
    return np.ascontiguousarray(np.stack([np.cos(ang) / 8.0, np.sin(ang) / 8.0], 1).astype(np.float32))


def a1b_inputs(hT_pad, w_in_l, scw_l, fnet_w_l, hh, ropec, ropes, c64):
    hs = slice(hh * 128, (hh + 1) * 128)
    hy_cols = np.concatenate([np.arange(part * 256 + hh * 128, part * 256 + hh * 128 + 128) for part in range(3)])
    heads = np.arange(4 * hh, 4 * hh + 4)
    qc = np.concatenate([Q_OFF + h * 64 + np.arange(64) for h in heads])
    kc = np.concatenate([K_OFF + h * 64 + np.arange(64) for h in heads])
    vc = np.concatenate([V_OFF + h * 64 + np.arange(64) for h in heads])
    w = np.concatenate([w_in_l[:, hy_cols], w_in_l[:, qc], w_in_l[:, qc ^ 1], w_in_l[:, kc], w_in_l[:, kc ^ 1], w_in_l[:, vc]], 1)
    fcols = F_OFF + (2 * hh) * 64 + np.arange(128)
    wfT = np.ascontiguousarray(w_in_l[:, fcols].T.reshape(2, 64, 1024).transpose(1, 0, 2))
    fw = np.ascontiguousarray(fnet_w_l[2 * hh:2 * hh + 2].transpose(1, 0, 2))
    return {"hT": hT_pad, "w": np.ascontiguousarray(w), "scw": bc128(scw_l[:, hy_cols]), "wfT": wfT, "fw": fw,
            "c64": c64, "ropec": ropec, "ropes": ropes}


def pad_hT(h_main, h_ctx):
    out = np.zeros((1024, 4356), ml_dtypes.bfloat16)
    out[:, 1:4097] = h_main.T
    out[:, 4099:4355] = h_ctx.T
    return out


def build_A3(c=None):
    c = c or Ctx()
    S = c.S
    qT = c.din("qT", [256, 4352], BF16)
    kT = c.din("kT", [256, 4352], BF16)
    V = c.din("V", [4352, 256], BF16)
    lamv = c.din("lamv", [128, 4, 32])
    lami = c.din("lami", [128, 2])
    subw = c.din("subw", [128, 64])
    att = c.dout("att", [4352, 256])
    qTs = c.sb("qTs", [64, 4, 4352], BF16)
    kTs = c.sb("kTs", [64, 4, 4352], BF16)
    Vs = c.sb("Vs", [128, 34, 4, 65], BF16)
    osb = c.sb("osb", [128, 34, 256], F32)
    lvs = c.sb("lvs", [128, 4, 32])
    lis = c.sb("lis", [128, 2])
    sws = c.sb("sws", [128, 64])
    epsb = make_eps(c)
    for rc in range(4):
        c.load("sync", qTs[:, rc, :], qT[rc * 64:(rc + 1) * 64, :], "qTs")
        c.load("sync", kTs[:, rc, :], kT[rc * 64:(rc + 1) * 64, :], "kTs")
    Vv = V.rearrange("(t p) (h d) -> p t h d", p=128, h=4)
    for t in range(34):
        c.load("sync", Vs[:, t, :, 0:64], Vv[:, t, :, :], "Vs")
    S.op("vector", lambda e: e.memset(Vs[:, :, :, 64:65], 1.0), writes=["Vs"])
    c.load("sync", lvs, lamv, "lvs")
    c.load("sync", lis, lami, "lis")
    c.load("sync", sws, subw, "sws")
    pr = c.sb("lpr", [128, 2, 32])
    ssum = c.sb("lss", [128, 2])
    ex = c.sb("lex", [128, 2])
    nlam = c.sb("nlam", [128, 1])
    S.op("vector", [lambda e: e.tensor_tensor(out=pr[:, 0, :], in0=lvs[:, 0, :], in1=lvs[:, 1, :], op=ALU.mult),
                    lambda e: e.tensor_tensor(out=pr[:, 1, :], in0=lvs[:, 2, :], in1=lvs[:, 3, :], op=ALU.mult)], reads=["lvs"], writes=["lpr"])
    S.op("vector", lambda e: e.reduce_sum(out=ssum, in_=pr, axis=AX.X), reads=["lpr"], writes=["lss"])
    S.op("scalar", lambda e: e.activation(out=ex, in_=ssum, func=AF.Exp), reads=["lss"], writes=["lex"])
    S.op("vector", lambda e: e.tensor_tensor(out=nlam, in0=ex[:, 1:2], in1=ex[:, 0:1], op=ALU.subtract), reads=["lex"], writes=["nlam"])
    S.op("vector", lambda e: e.tensor_tensor(out=nlam, in0=nlam, in1=lis[:, 0:1], op=ALU.subtract), reads=["nlam", "lis"], writes=["nlam"])
    S.op("vector", lambda e: e.tensor_scalar(out=sws, in0=sws, scalar1=lis[:, 1:2], scalar2=None, op0=ALU.mult), reads=["sws", "lis"], writes=["sws"])
    sc_banks = [(c.ps[0], ("ps", 0)), (c.ps[1], ("ps", 1))]
    acc_banks = [(c.ps[2 + i], ("ps", 2 + i)) for i in range(4)]
    scale = 1.0 / math.sqrt(32.0)
    it = 0
    jobs = [(q0, 512, list(range(34))) for q0 in range(0, 4096, 512)] + [(4096, 256, [32, 33])]
    for hl in range(4):
        rc = hl
        for (q0, nq, ktiles) in jobs:
            nsub = nq // 128
            ocs = []
            for cm in range(2):
                pb = cm * 32
                for ki, kt in enumerate(ktiles):
                    sp, spk = sc_banks[it % 2]
                    E, Ek = c.rotbuf("E", [128, 512], BF16, 3)
                    it += 1
                    S.op("tensor", lambda e, sp=sp, pb=pb, rc=rc, kt=kt, q0=q0, nq=nq: e.matmul(
                        sp[:, 0:nq], lhsT=kTs[pb:pb + 32, rc, kt * 128:(kt + 1) * 128], rhs=qTs[pb:pb + 32, rc, q0:q0 + nq], start=True, stop=True),
                        reads=["qTs", "kTs"], writes=[spk])
                    S.op("scalar", lambda e, sp=sp, E=E, nq=nq: e.activation(out=E[:, 0:nq], in_=sp[:, 0:nq], func=AF.Exp, scale=scale),
                         reads=[spk], writes=[Ek])
                    S.op("tensor", [(lambda e, E=E, s=s, kt=kt, hl=hl, ki=ki, nk=len(ktiles): e.matmul(
                        acc_banks[s][0][:, 0:65], lhsT=E[:, s * 128:(s + 1) * 128], rhs=Vs[:, kt, hl, :], start=(ki == 0), stop=(ki == nk - 1)))
                        for s in range(nsub)], reads=[Ek, "Vs"], writes=[acc_banks[s][1] for s in range(nsub)])
                oc, ock = c.rotbuf("oc", [128, 4, 64], F32, 4)
                rcp, rcpk = c.rotbuf("rcp", [128, 4], F32, 4)
                for s in range(nsub):
                    ab, abk = acc_banks[s]
                    S.op("vector", lambda e, ab=ab, rcp=rcp, s=s: e.reciprocal(out=rcp[:, s:s + 1], in_=ab[:, 64:65]), reads=[abk], writes=[rcpk])
                    S.op("vector", lambda e, ab=ab, rcp=rcp, oc=oc, s=s: e.tensor_scalar(out=oc[:, s, :], in0=ab[:, 0:64], scalar1=rcp[:, s:s + 1], scalar2=None, op0=ALU.mult),
                         reads=[abk, rcpk], writes=[ock])
                ocs.append((oc, ock))
            (o0, o0k), (o1, o1k) = ocs
            od, odk = c.rotbuf("od", [128, 4, 64], F32, 2)
            sq, sqk = c.rotbuf("sq", [128, 4, 64], F32, 2)
            ss, ssk = c.rotbuf("ss", [128, 4], F32, 2)
            sd, sdk = c.rotbuf("asd", [128, 4], F32, 2)
            rs, rsk = c.rotbuf("ars", [128, 4], F32, 2)
            S.op("vector", lambda e, o0=o0, o1=o1, od=od, nsub=nsub: e.scalar_tensor_tensor(
                out=od[:, 0:nsub, :], in0=o1[:, 0:nsub, :], scalar=nlam[:, 0:1], in1=o0[:, 0:nsub, :], op0=ALU.mult, op1=ALU.add),
                reads=[o0k, o1k, "nlam"], writes=[odk])
            S.op("gpsimd", lambda e, od=od, sq=sq, nsub=nsub: e.tensor_tensor(out=sq[:, 0:nsub, :], in0=od[:, 0:nsub, :], in1=od[:, 0:nsub, :], op=ALU.mult),
                 reads=[odk], writes=[sqk])
            S.op("vector", lambda e, sq=sq, ss=ss, nsub=nsub: e.reduce_sum(out=ss[:, 0:nsub], in_=sq[:, 0:nsub, :], axis=AX.X), reads=[sqk], writes=[ssk])
            S.op("scalar", lambda e, ss=ss, sd=sd, nsub=nsub: e.activation(out=sd[:, 0:nsub], in_=ss[:, 0:nsub], func=AF.Sqrt, bias=epsb[:, 0:1], scale=1.0 / 64.0),
                 reads=[ssk, "epsb"], writes=[sdk])
            S.op("vector", lambda e, sd=sd, rs=rs, nsub=nsub: e.reciprocal(out=rs[:, 0:nsub], in_=sd[:, 0:nsub]), reads=[sdk], writes=[rsk])
            for s in range(nsub):
                tt = q0 // 128 + s
                S.op("vector", lambda e, od=od, rs=rs, s=s, tt=tt, hl=hl: e.scalar_tensor_tensor(
                    out=osb[:, tt, hl * 64:(hl + 1) * 64], in0=od[:, s, :], scalar=rs[:, s:s + 1], in1=sws, op0=ALU.mult, op1=ALU.mult),
                    reads=[odk, rsk, "sws"], writes=["osb"])
    av = att.rearrange("(t p) c -> p t c", p=128)
    for t0 in range(0, 34, 17):
        c.store("sync", av[:, t0:t0 + 17, :], osb[:, t0:t0 + 17, :], "osb")
    return c


def lam_init_of(layer):
    return 0.8 - 0.6 * math.exp(-0.3 * layer)


def fft_consts():
    N, N1, N2 = 8192, 128, 64
    bf = ml_dtypes.bfloat16
    n1 = np.arange(64)[:, None]
    k1 = np.arange(128)[None, :]
    a = 2 * np.pi * n1 * k1 / N1
    D1 = np.stack([np.cos(a), -np.sin(a)], 1).astype(bf)
    n2 = np.arange(64)[:, None]
    t = 2 * np.pi * n2 * k1 / N
    tw = np.stack([np.cos(t), -np.sin(t)], 0)
    TW = np.tile(tw[:, None, :, :], (1, 2, 1, 1)).reshape(2, 128, 128)
    TW2 = np.ascontiguousarray(np.tile(TW[:, :, None, :], (1, 1, 2, 1)).transpose(1, 0, 2, 3)).astype(np.float32)
    j = np.arange(64)
    d = 2 * np.pi * np.outer(j, j) / N2
    kr = np.kron(np.eye(2), np.cos(d))
    ki = np.kron(np.eye(2), -np.sin(d))
    D2 = np.stack([kr, ki, -ki], 1).astype(bf)
    ti = 2 * np.pi * np.arange(128)[:, None] * np.arange(64)[None, :] / N
    twi = np.stack([np.cos(ti), np.sin(ti)], 1)
    TWI = np.ascontiguousarray(np.tile(twi[:, :, None, None, :], (1, 1, 2, 2, 1))).astype(np.float32)
    ai = 2 * np.pi * np.arange(128)[:, None] * np.arange(64)[None, :] / N1
    D1I = np.stack([np.cos(ai) / N, -np.sin(ai) / N], 1).astype(bf)
    return {"D1": D1, "TW2": TW2, "D2": D2, "TWI": TWI.reshape(128, 2, 2, 128), "D1I": D1I}


class FFT:
    def __init__(self, c, nk1=128):
        self.c = c
        self.nk1 = nk1
        self.D1d = c.din("D1", [64, 2, 128], BF16)
        self.TW2d = c.din("TW2", [128, 2, 2, 128])
        self.D2d = c.din("D2", [128, 3, 128], BF16)
        self.D1 = c.sb("fD1", [64, 2, 128], BF16)
        self.TW2 = c.sb("fTW2", [128, 2, 2, 128])
        self.D2 = c.sb("fD2", [128, 3, 128], BF16)
        c.load("sync", self.D1, self.D1d, "fD1")
        c.load("sync", self.TW2, self.TW2d, "fTW2")
        c.load("sync", self.D2, self.D2d, "fD2")

    def load_inv(self):
        c = self.c
        self.TWId = c.din("TWI", [128, 2, 2, 128])
        self.D1Id = c.din("D1I", [128, 2, 64], BF16)
        self.TWI = c.sb("fTWI", [128, 2, 2, 128])
        self.D1I = c.sb("fD1I", [128, 2, 64], BF16)
        c.load("sync", self.TWI, self.TWId, "fTWI")
        c.load("sync", self.D1I, self.D1Id, "fD1I")

    def cmul(self, re, im, cre, cim, ore, oim, rkeys, ckeys, okeys, shape):
        c, S = self.c, self.c.S
        ms = []
        for nm in ("cm1", "cm2", "cm3", "cm4"):
            m, mk = c.rotbuf(nm, [128, 256], F32, 2)
            ms.append((m[:, 0:shape[0] * shape[1]].rearrange("p (a b) -> p a b", a=shape[0]), mk))
        (m1, k1), (m2, k2), (m3, k3), (m4, k4) = ms
        S.op("vector", lambda e: e.tensor_tensor(out=m1, in0=re, in1=cre, op=ALU.mult), reads=rkeys + ckeys, writes=[k1])
        S.op("vector", lambda e: e.tensor_tensor(out=m2, in0=im, in1=cim, op=ALU.mult), reads=rkeys + ckeys, writes=[k2])
        S.op("gpsimd", lambda e: e.tensor_tensor(out=ore, in0=m1, in1=m2, op=ALU.subtract), reads=[k1, k2], writes=okeys)
        S.op("vector", lambda e: e.tensor_tensor(out=m3, in0=re, in1=cim, op=ALU.mult), reads=rkeys + ckeys, writes=[k3])
        S.op("vector", lambda e: e.tensor_tensor(out=m4, in0=im, in1=cre, op=ALU.mult), reads=rkeys + ckeys, writes=[k4])
        S.op("gpsimd", lambda e: e.tensor_tensor(out=oim, in0=m3, in1=m4, op=ALU.add), reads=[k3, k4], writes=okeys)

    def fwd2(self, xs, xkeys, pa):
        c, S, nk1 = self.c, self.c.S, self.nk1
        step = 128 // nk1
        ps, pk = c.psum()
        v1 = ps[:, 0:4 * nk1].rearrange("p (a t k) -> p a t k", a=2, t=2)
        S.op("tensor", [(lambda e, a=a, t=t: e.matmul(v1[:, a, t, :], lhsT=xs[:, 2 * (pa + a):2 * (pa + a) + 2, :],
                                                      rhs=self.D1[:, t, 0:128:step], start=True, stop=True)) for a in range(2) for t in range(2)],
             reads=list(xkeys) + ["fD1"], writes=[pk])
        Z, Zk = c.rotbuf("fZ", [128, 2, 2, 128], BF16, 2)
        self.cmul(v1[:, :, 0, :], v1[:, :, 1, :], self.TW2[:, 0, :, 0:128:step], self.TW2[:, 1, :, 0:128:step],
                  Z[:, 0, :, 0:nk1], Z[:, 1, :, 0:nk1], [pk], ["fTW2"], [Zk], (2, nk1))
        ps2, pk2 = c.psum()
        v2 = ps2[:, 0:4 * nk1].rearrange("p (a t k) -> p a t k", a=2, t=2)
        mm = []
        for a in range(2):
            mm.append(lambda e, a=a: e.matmul(v2[:, a, 0, :], lhsT=self.D2[:, 0, :], rhs=Z[:, 0, a, 0:nk1], start=True, stop=False))
            mm.append(lambda e, a=a: e.matmul(v2[:, a, 0, :], lhsT=self.D2[:, 2, :], rhs=Z[:, 1, a, 0:nk1], start=False, stop=True))
            mm.append(lambda e, a=a: e.matmul(v2[:, a, 1, :], lhsT=self.D2[:, 0, :], rhs=Z[:, 1, a, 0:nk1], start=True, stop=False))
            mm.append(lambda e, a=a: e.matmul(v2[:, a, 1, :], lhsT=self.D2[:, 1, :], rhs=Z[:, 0, a, 0:nk1], start=False, stop=True))
        S.op("tensor", mm, reads=[Zk, "fD2"], writes=[pk2])
        return v2, pk2

    def inv2(self, Y, Yk, out_view, okey):
        c, S = self.c, self.c.S
        ps, pk = c.psum()
        v = ps[:, 0:512].rearrange("p (a t k) -> p a t k", a=2, t=2)
        mm = []
        for a in range(2):
            mm.append(lambda e, a=a: e.matmul(v[:, a, 0, :], lhsT=Y[:, 0, a, :], rhs=self.D2[:, 0, :], start=True, stop=False))
            mm.append(lambda e, a=a: e.matmul(v[:, a, 0, :], lhsT=Y[:, 1, a, :], rhs=self.D2[:, 1, :], start=False, stop=True))
            mm.append(lambda e, a=a: e.matmul(v[:, a, 1, :], lhsT=Y[:, 0, a, :], rhs=self.D2[:, 2, :], start=True, stop=False))
            mm.append(lambda e, a=a: e.matmul(v[:, a, 1, :], lhsT=Y[:, 1, a, :], rhs=self.D2[:, 0, :], start=False, stop=True))
        S.op("tensor", mm, reads=[Yk, "fD2"], writes=[pk])
        Zi, Zik = c.rotbuf("fZi", [128, 2, 2, 128], BF16, 2)
        self.cmul(v[:, :, 0, :], v[:, :, 1, :], self.TWI[:, 0, :, :], self.TWI[:, 1, :, :], Zi[:, 0, :, :], Zi[:, 1, :, :],
                  [pk], ["fTWI"], [Zik], (2, 128))
        S.op("tensor", [lambda e: e.matmul(out_view, lhsT=self.D1I[:, 0, :], rhs=Zi[:, 0, :, :], start=True, stop=False),
                        lambda e: e.matmul(out_view, lhsT=self.D1I[:, 1, :], rhs=Zi[:, 1, :, :], start=False, stop=True)],
             reads=[Zik, "fD1I"], writes=[okey])


PI = math.pi


def filt_mlp(c, zT_ap, n, w1s, w2s, fv, a2T, a2key, tag):
    S = c.S
    cur_in = None
    for c0 in range(0, n, 512):
        m = min(512, n - c0)
        zt, zk = c.rotbuf(tag + "zt", [33, 512], F32, 2)
        c.load("sync", zt[:, 0:m], zT_ap[:, c0:c0 + m], zk)
        src, skey, wgt, bcol = zt, zk, w1s, 1
        for layer in range(2):
            ps, pk = c.psum()
            S.op("tensor", lambda e, ps=ps, wgt=wgt, src=src, m=m: e.matmul(ps[0:64, 0:m], lhsT=wgt, rhs=src[:, 0:m], start=True, stop=True),
                 reads=[skey, "fw12"], writes=[pk])
            ar, ak = c.rotbuf(tag + "ar", [64, 512], F32, 2)
            mk_, mkk = c.rotbuf(tag + "mk", [64, 512], F32, 2)
            S.op("vector", lambda e, ps=ps, ar=ar, m=m, bcol=bcol: e.tensor_scalar(out=ar[:, 0:m], in0=ps[0:64, 0:m], scalar1=fv[:, bcol:bcol + 1], scalar2=fv[:, 0:1], op0=ALU.add, op1=ALU.mult),
                 reads=[pk, "fv"], writes=[ak])
            S.op("vector", lambda e, ar=ar, mk_=mk_, m=m: e.tensor_scalar(out=mk_[:, 0:m], in0=ar[:, 0:m], scalar1=PI, scalar2=-2 * PI, op0=ALU.is_gt, op1=ALU.mult), reads=[ak], writes=[mkk])
            S.op("vector", lambda e, ar=ar, mk_=mk_, m=m: e.tensor_tensor(out=ar[:, 0:m], in0=ar[:, 0:m], in1=mk_[:, 0:m], op=ALU.add), reads=[ak, mkk], writes=[ak])
            S.op("vector", lambda e, ar=ar, mk_=mk_, m=m: e.tensor_scalar(out=mk_[:, 0:m], in0=ar[:, 0:m], scalar1=-PI, scalar2=2 * PI, op0=ALU.is_lt, op1=ALU.mult), reads=[ak], writes=[mkk])
            S.op("vector", lambda e, ar=ar, mk_=mk_, m=m: e.tensor_tensor(out=ar[:, 0:m], in0=ar[:, 0:m], in1=mk_[:, 0:m], op=ALU.add), reads=[ak, mkk], writes=[ak])
            if layer == 0:
                a1, a1k = c.rotbuf(tag + "a1", [64, 512], F32, 2)
                S.op("scalar", lambda e, ar=ar, a1=a1, m=m: e.activation(out=a1[:, 0:m], in_=ar[:, 0:m], func=AF.Sin), reads=[ak], writes=[a1k])
                src, skey, wgt, bcol = a1, a1k, w2s, 2
            else:
                S.op("scalar", lambda e, ar=ar, m=m, c0=c0: e.activation(out=a2T[:, c0:c0 + m], in_=ar[:, 0:m], func=AF.Sin), reads=[ak], writes=[a2key])


def build_A2(c=None):
    c = c or Ctx()
    S = c.S
    hyXd = c.din("hyX", [64, 384, 64], BF16)
    hycd = c.din("hyc", [256, 384], BF16)
    zTd = c.din("zT", [33, 4096])
    zTcd = c.din("zTc", [33, 256])
    w1d = c.din("w1", [33, 64])
    w2d = c.din("w2", [64, 64])
    fvd = c.din("fv", [64, 3])
    w3d = c.din("w3", [64, 512])
    wind = c.din("win", [64, 128, 64], BF16)
    wincd = c.din("winc", [128, 2, 128])
    hbd = c.din("hb", [128, 2, 128])
    Ccd = c.din("Cc", [128, 2, 512], BF16)
    Scd = c.din("Sc", [128, 2, 512], BF16)
    Gcd = c.din("Gc", [128, 4, 256], BF16)
    Gsd = c.din("Gs", [128, 4, 256], BF16)
    zXo = c.dout("zX", [64, 128, 64])
    zco = c.dout("zc", [256, 128])
    F = FFT(c)
    F.load_inv()
    w1s = c.sb("w1s", [33, 64]); w2s = c.sb("w2s", [64, 64]); fv = c.sb("fvs", [64, 3]); w3s = c.sb("w3s", [64, 512])
    wins = c.sb("wins", [64, 128, 64], BF16); hbs = c.sb("hbs", [128, 2, 128])
    ones = c.sb("ones", [128, 128])
    a2T = c.sb("a2T", [64, 4096]); a2Tc = c.sb("a2Tc", [64, 256])
    for (d, s, k) in ((w1d, w1s, "fw12"), (w2d, w2s, "fw12"), (fvd, fv, "fv"), (w3d, w3s, "w3s"), (wind, wins, "wins"), (hbd, hbs, "hbs")):
        c.load("sync", s, d, k)
    S.op("vector", lambda e: e.memset(ones, 1.0), writes=["ones"])
    filt_mlp(c, zTd, 4096, w1s, w2s, fv, a2T, "a2T", "m")
    filt_mlp(c, zTcd, 256, w1s, w2s, fv, a2Tc, "a2Tc", "m")

    hX = c.sb("hXo", [64, 2, 128, 64], BF16)
    Kre = c.sb("Kre", [128, 32, 128], BF16); Kim = c.sb("Kim", [128, 32, 128], BF16)
    zA = c.sb("zA", [64, 128, 64], BF16); gA = c.sb("gA", [64, 128, 64], BF16)
    c.load("sync", zA, hyXd[:, 256:384, :], "zA")
    cur, curk, nxt, nxtk = zA, "zA", zA, "zA"
    for o in range(2):
        c.load("sync", gA, hyXd[:, o * 128:(o + 1) * 128, :], "gA")
        acc = c.sb(f"l1acc{o}", [64, 256])
        S.op("vector", lambda e, acc=acc: e.memset(acc, 0.0), writes=[f"l1acc{o}"])
        for n2 in range(64):
            ps, pk = c.psum()
            S.op("tensor", lambda e, ps=ps, n2=n2, o=o: e.matmul(ps[0:64, 0:256], lhsT=a2T[:, n2:4096:64], rhs=w3s[:, o * 256:(o + 1) * 256], start=True, stop=True),
                 reads=["a2T", "w3s"], writes=[pk])
            t, tk = c.rotbuf("l1t", [64, 2, 128], F32, 2)
            S.op("vector", [lambda e, ps=ps, t=t, n2=n2: e.tensor_tensor(out=t[:, 0, :], in0=ps[0:64, 0:128], in1=wins[:, :, n2], op=ALU.mult),
                            lambda e, ps=ps, t=t, n2=n2: e.tensor_tensor(out=t[:, 1, :], in0=ps[0:64, 128:256], in1=wins[:, :, n2], op=ALU.mult)],
                 reads=[pk, "wins"], writes=[tk])
            if n2 == 0:
                S.op("vector", lambda e, t=t: e.memset(t[0:1, 1, :], 0.0), reads=[tk], writes=[tk])
            S.op("scalar", lambda e, t=t: e.activation(out=t, in_=t, func=AF.Abs), reads=[tk], writes=[tk])
            S.op("vector", lambda e, t=t, acc=acc: e.tensor_tensor(out=acc, in0=t.rearrange("p a b -> p (a b)"), in1=acc, op=ALU.add),
                 reads=[tk, f"l1acc{o}"], writes=[f"l1acc{o}"])
        l1p = c.sb(f"l1p{o}", [64, 128]); rl1 = c.sb(f"rl1{o}", [64, 128]); w3n = c.sb(f"w3n{o}", [64, 256])
        S.op("vector", lambda e, acc=acc, l1p=l1p: e.tensor_tensor(out=l1p, in0=acc[:, 0:128], in1=acc[:, 128:256], op=ALU.add), reads=[f"l1acc{o}"], writes=[f"l1p{o}"])
        ps, pk = c.psum()
        S.op("tensor", lambda e, ps=ps, l1p=l1p: e.matmul(ps[0:64, 0:128], lhsT=ones[0:64, 0:64], rhs=l1p, start=True, stop=True), reads=[f"l1p{o}", "ones"], writes=[pk])
        S.op("vector", lambda e, ps=ps, rl1=rl1: e.reciprocal(out=rl1, in_=ps[0:64, 0:128]), reads=[pk], writes=[f"rl1{o}"])
        S.op("vector", [lambda e, w3n=w3n, rl1=rl1, o=o, d=d: e.tensor_tensor(out=w3n[:, d * 128:(d + 1) * 128], in0=w3s[:, o * 256 + d * 128:o * 256 + (d + 1) * 128], in1=rl1, op=ALU.mult) for d in range(2)],
             reads=["w3s", f"rl1{o}"], writes=[f"w3n{o}"])
        for n2 in range(64):
            ps, pk = c.psum()
            S.op("tensor", lambda e, ps=ps, n2=n2, w3n=w3n: e.matmul(ps[0:64, 0:256], lhsT=a2T[:, n2:4096:64], rhs=w3n, start=True, stop=True),
                 reads=["a2T", f"w3n{o}"], writes=[pk])
            S.op("vector", [lambda e, ps=ps, n2=n2, d=d: e.tensor_tensor(out=hX[:, d, :, n2], in0=ps[0:64, d * 128:(d + 1) * 128], in1=wins[:, :, n2], op=ALU.mult) for d in range(2)],
                 reads=[pk, "wins"], writes=["hX"])
        S.op("vector", lambda e: e.memset(hX[0:1, 1, :, 0:1], 0.0), reads=["hX"], writes=["hX"])
        S.op("vector", lambda e, o=o: e.tensor_tensor(out=hX[0:1, 0, :, 0], in0=hX[0:1, 0, :, 0], in1=hbs[0:1, o, :], op=ALU.add), reads=["hX", "hbs"], writes=["hX"])
        for hf2 in range(2):
            for pa in range(32 * hf2, 32 * hf2 + 32, 2):
                vf, kf = F.fwd2(hX[:, 0, :, :], ["hX"], pa)
                vb, kb = F.fwd2(hX[:, 1, :, :], ["hX"], pa)
                tb, tbk = c.rotbuf("Ktb", [128, 2, 2, 128], F32, 1)
                pl = pa - 32 * hf2
                S.op("scalar", lambda e, vb=vb, tb=tb: e.copy(out=tb, in_=vb), reads=[kb], writes=[tbk])
                S.op("vector", lambda e, vf=vf, tb=tb, pl=pl: e.tensor_tensor(out=Kre[:, pl:pl + 2, :], in0=vf[:, :, 0, :], in1=tb[:, :, 0, :], op=ALU.add), reads=[kf, tbk], writes=["K"])
                S.op("vector", lambda e, vf=vf, tb=tb, pl=pl: e.tensor_tensor(out=Kim[:, pl:pl + 2, :], in0=vf[:, :, 1, :], in1=tb[:, :, 1, :], op=ALU.subtract), reads=[kf, tbk], writes=["K"])
            for pa in range(32 * hf2, 32 * hf2 + 32, 4):
                pso, psok = c.psum()
                for h2 in range(2):
                    p0 = pa + 2 * h2
                    pl = p0 - 32 * hf2
                    vu, ku = F.fwd2(cur, [curk], p0)
                    Y, Yk = c.rotbuf("fY", [128, 2, 2, 128], BF16, 2)
                    F.cmul(vu[:, :, 0, :], vu[:, :, 1, :], Kre[:, pl:pl + 2, :], Kim[:, pl:pl + 2, :], Y[:, 0, :, :], Y[:, 1, :, :], [ku], ["K"], [Yk], (2, 128))
                    F.inv2(Y, Yk, pso[0:64, h2 * 256:(h2 + 1) * 256], psok)
                c0 = 2 * pa
                if o == 0:
                    S.op("vector", lambda e, pso=pso, c0=c0, nxt=nxt: e.tensor_tensor(out=nxt[:, c0:c0 + 8, :], in0=pso[0:64, 0:512].rearrange("p (a b) -> p a b", a=8), in1=gA[:, c0:c0 + 8, :], op=ALU.mult),
                         reads=[psok, "gA"], writes=[nxtk])
                else:
                    zo, zok = c.rotbuf("zo", [64, 8, 64], F32, 2)
                    S.op("vector", lambda e, pso=pso, c0=c0, zo=zo: e.tensor_tensor(out=zo, in0=pso[0:64, 0:512].rearrange("p (a b) -> p a b", a=8), in1=gA[:, c0:c0 + 8, :], op=ALU.mult),
                         reads=[psok, "gA"], writes=[zok])
                    c.store("sync", zXo[:, c0:c0 + 8, :], zo, zok)
        cur, curk, nxt, nxtk = nxt, nxtk, cur, curk

    Cc = c.sb("Ccs", [128, 2, 512], BF16); Sc = c.sb("Scs", [128, 2, 512], BF16)
    Gc = c.sb("Gcs", [128, 4, 256], BF16); Gs = c.sb("Gss", [128, 4, 256], BF16)
    winc = c.sb("wincs", [128, 2, 128]); hyc = c.sb("hycs", [128, 2, 384], BF16)
    for (d, s, k) in ((Ccd, Cc, "Cc"), (Scd, Sc, "Sc"), (Gcd, Gc, "Gc"), (Gsd, Gs, "Gs"), (wincd, winc, "winc")):
        c.load("sync", s, d, k)
    c.load("sync", hyc, hycd.rearrange("(t p) n -> p t n", p=128), "hyc")
    hc = c.sb("hcf", [128, 2, 512])
    accc = c.sb("accc", [128, 512])
    for tl in range(2):
        ps, pk = c.psum()
        S.op("tensor", lambda e, ps=ps, tl=tl: e.matmul(ps[:, 0:512], lhsT=a2Tc[:, tl * 128:(tl + 1) * 128], rhs=w3s, start=True, stop=True), reads=["a2Tc", "w3s"], writes=[pk])
        S.op("vector", [lambda e, ps=ps, tl=tl, q=q: e.tensor_tensor(out=hc[:, tl, q * 128:(q + 1) * 128], in0=ps[:, q * 128:(q + 1) * 128], in1=winc[:, tl, :], op=ALU.mult) for q in range(4)],
             reads=[pk, "winc"], writes=["hc"])
    for o in range(2):
        S.op("vector", lambda e, o=o: e.memset(hc[0:1, 0, o * 256 + 128:o * 256 + 256], 0.0), reads=["hc"], writes=["hc"])
    habs = c.sb("habs", [128, 2, 512])
    S.op("scalar", lambda e: e.activation(out=habs, in_=hc, func=AF.Abs), reads=["hc"], writes=["habs"])
    S.op("vector", lambda e: e.tensor_tensor(out=accc, in0=habs[:, 0, :], in1=habs[:, 1, :], op=ALU.add), reads=["habs"], writes=["accc"])
    ps, pk = c.psum()
    S.op("tensor", lambda e, ps=ps: e.matmul(ps[:, 0:512], lhsT=ones, rhs=accc, start=True, stop=True), reads=["accc", "ones"], writes=[pk])
    l1c = c.sb("l1c", [128, 2, 128]); rl1c = c.sb("rl1c", [128, 2, 128])
    S.op("vector", lambda e, ps=ps: e.tensor_copy(out=accc, in_=ps[:, 0:512]), reads=[pk], writes=["accc"])
    l1v = accc.rearrange("p (o d c) -> p o d c", o=2, d=2)
    S.op("vector", lambda e: e.tensor_tensor(out=l1c, in0=l1v[:, :, 0, :], in1=l1v[:, :, 1, :], op=ALU.add), reads=["accc"], writes=["l1c"])
    S.op("vector", lambda e: e.reciprocal(out=rl1c, in_=l1c), reads=["l1c"], writes=["rl1c"])
    hs = c.sb("hsum", [128, 2, 2, 128], BF16); hd = c.sb("hdif", [128, 2, 2, 128], BF16)
    hcv = hc.rearrange("p t (o d c) -> p t o d c", o=2, d=2)
    for tl in range(2):
        for d in range(2):
            S.op("vector", lambda e, tl=tl, d=d: e.tensor_tensor(out=hcv[:, tl, :, d, :], in0=hcv[:, tl, :, d, :], in1=rl1c, op=ALU.mult), reads=["hc", "rl1c"], writes=["hc"])
    S.op("vector", lambda e: e.tensor_tensor(out=hcv[0:1, 0, :, 0, :], in0=hcv[0:1, 0, :, 0, :], in1=hbs[0:1, :, :], op=ALU.add), reads=["hc", "hbs"], writes=["hc"])
    for tl in range(2):
        S.op("vector", lambda e, tl=tl: e.tensor_tensor(out=hs[:, tl, :, :], in0=hcv[:, tl, :, 0, :], in1=hcv[:, tl, :, 1, :], op=ALU.add), reads=["hc"], writes=["hs"])
        S.op("vector", lambda e, tl=tl: e.tensor_tensor(out=hd[:, tl, :, :], in0=hcv[:, tl, :, 1, :], in1=hcv[:, tl, :, 0, :], op=ALU.subtract), reads=["hc"], writes=["hd"])
    KA = c.sb("KA", [128, 4, 256], BF16); KB = c.sb("KB", [128, 4, 256], BF16)
    for fc in range(4):
        for (mat, src, skey, dst, dk) in ((Cc, hs, "hs", KA, "KA"), (Sc, hd, "hd", KB, "KB")):
            ps, pk = c.psum()
            S.op("tensor", [lambda e, ps=ps, mat=mat, src=src, fc=fc, tl=tl: e.matmul(ps[:, 0:256], lhsT=mat[:, tl, fc * 128:(fc + 1) * 128], rhs=src[:, tl, :, :], start=(tl == 0), stop=(tl == 1)) for tl in range(2)],
                 reads=[skey, "Cc", "Sc"], writes=[pk])
            S.op("vector", lambda e, ps=ps, dst=dst, fc=fc: e.tensor_copy(out=dst[:, fc, :], in_=ps[:, 0:256]), reads=[pk], writes=[dk])
    uc = c.sb("ucur", [128, 2, 128], BF16)
    uc2 = c.sb("uc2", [128, 2, 128], BF16)
    uck = "uc"
    uc0 = uc
    Ys = {}
    S.op("vector", lambda e, uc=uc: e.tensor_copy(out=uc, in_=hyc[:, :, 256:384]), reads=["hyc"], writes=["uc"])
    for o in range(2):
        Yre = c.sb(f"cYre{o}", [128, 4, 128], BF16); Yim = c.sb(f"cYim{o}", [128, 4, 128], BF16)
        Ys[o] = Yre
        for fc in range(4):
            psA, pkA = c.psum()
            S.op("tensor", [lambda e, psA=psA, fc=fc, tl=tl, uc=uc: e.matmul(psA[:, 0:128], lhsT=Cc[:, tl, fc * 128:(fc + 1) * 128], rhs=uc[:, tl, :], start=(tl == 0), stop=(tl == 1)) for tl in range(2)],
                 reads=[uck, "Cc"], writes=[pkA])
            psB, pkB = c.psum()
            S.op("tensor", [lambda e, psB=psB, fc=fc, tl=tl, uc=uc: e.matmul(psB[:, 0:128], lhsT=Sc[:, tl, fc * 128:(fc + 1) * 128], rhs=uc[:, tl, :], start=(tl == 0), stop=(tl == 1)) for tl in range(2)],
                 reads=[uck, "Sc"], writes=[pkB])
            ms = [c.rotbuf(f"cy{i}", [128, 128], F32, 1) for i in range(4)]
            ka = KA[:, fc, o * 128:(o + 1) * 128]; kb_ = KB[:, fc, o * 128:(o + 1) * 128]
            S.op("vector", lambda e, psA=psA, m=ms[0][0], ka=ka: e.tensor_tensor(out=m, in0=psA[:, 0:128], in1=ka, op=ALU.mult), reads=[pkA, "KA"], writes=[ms[0][1]])
            S.op("vector", lambda e, psB=psB, m=ms[1][0], kb_=kb_: e.tensor_tensor(out=m, in0=psB[:, 0:128], in1=kb_, op=ALU.mult), reads=[pkB, "KB"], writes=[ms[1][1]])
            S.op("gpsimd", lambda e, a=ms[0][0], b=ms[1][0], Yre=Yre, fc=fc: e.tensor_tensor(out=Yre[:, fc, :], in0=a, in1=b, op=ALU.add), reads=[ms[0][1], ms[1][1]], writes=[f"cY{o}"])
            S.op("vector", lambda e, psA=psA, m=ms[2][0], kb_=kb_: e.tensor_tensor(out=m, in0=psA[:, 0:128], in1=kb_, op=ALU.mult), reads=[pkA, "KB"], writes=[ms[2][1]])
            S.op("vector", lambda e, psB=psB, m=ms[3][0], ka=ka: e.tensor_tensor(out=m, in0=psB[:, 0:128], in1=ka, op=ALU.mult), reads=[pkB, "KA"], writes=[ms[3][1]])
            S.op("gpsimd", lambda e, a=ms[2][0], b=ms[3][0], Yim=Yim, fc=fc: e.tensor_tensor(out=Yim[:, fc, :], in0=a, in1=b, op=ALU.subtract), reads=[ms[2][1], ms[3][1]], writes=[f"cY{o}"])
        for tl in range(2):
            ps, pk = c.psum()
            mm = []
            for fc in range(4):
                mm.append(lambda e, ps=ps, fc=fc, tl=tl, Yre=Yre: e.matmul(ps[:, 0:128], lhsT=Gc[:, fc, tl * 128:(tl + 1) * 128], rhs=Yre[:, fc, :], start=(fc == 0), stop=False))
                mm.append(lambda e, ps=ps, fc=fc, tl=tl, Yim=Yim: e.matmul(ps[:, 0:128], lhsT=Gs[:, fc, tl * 128:(tl + 1) * 128], rhs=Yim[:, fc, :], start=False, stop=(fc == 3)))
            S.op("tensor", mm, reads=[f"cY{o}", "Gc", "Gs"], writes=[pk])
            if o == 0:
                S.op("vector", lambda e, ps=ps, tl=tl: e.tensor_tensor(out=uc2[:, tl, :], in0=ps[:, 0:128], in1=hyc[:, tl, 0:128], op=ALU.mult), reads=[pk, "hyc"], writes=["uc2"])
            else:
                zo, zok = c.rotbuf("zco", [128, 128], F32, 2)
                S.op("vector", lambda e, ps=ps, tl=tl, zo=zo: e.tensor_tensor(out=zo, in0=ps[:, 0:128], in1=hyc[:, tl, 128:256], op=ALU.mult), reads=[pk, "hyc"], writes=[zok])
                c.store("sync", zco[tl * 128:(tl + 1) * 128, :], zo, zok)
        uc, uck = uc2, "uc2"
    return c


HY_MIN_DECAY = math.log(1e-2) / 1.5
HY_MAX_DECAY = math.log(1e-2) / 0.3


def hyena_consts():
    bf = ml_dtypes.bfloat16
    out = {}
    for L, nm in ((4096, "zT"), (256, "zTc")):
        t = np.linspace(0.0, 1.0, L, dtype=np.float32)[:, None]
        omega = (np.float32(2.0 * math.pi / L) * np.arange(L, dtype=np.float32))[:, None]
        fb = np.linspace(1e-4, 15, 16, dtype=np.float32)[None, :]
        z = np.concatenate([t, np.cos(fb * omega), -np.sin(fb * omega)], -1).astype(np.float32)
        out[nm] = np.ascontiguousarray(z.T)
    deltas = np.abs(np.linspace(HY_MIN_DECAY, HY_MAX_DECAY, 256, dtype=np.float32))
    t = np.linspace(0.0, 1.0, 4096, dtype=np.float32)
    w = np.exp(-t[:, None] * deltas[None, :]).astype(np.float32)
    out["win_full"] = w.reshape(64, 64, 256).transpose(0, 2, 1)
    tc = np.linspace(0.0, 1.0, 256, dtype=np.float32)
    wc = np.exp(-tc[:, None] * deltas[None, :]).astype(np.float32)
    out["winc_full"] = wc.reshape(2, 128, 256).transpose(1, 0, 2)
    s = np.arange(256)[:, None]; f = np.arange(512)[None, :]
    a = 2 * np.pi * s * f / 512
    out["Cc"] = np.ascontiguousarray(np.cos(a).reshape(2, 128, 512).transpose(1, 0, 2)).astype(bf)
    out["Sc"] = np.ascontiguousarray(np.sin(a).reshape(2, 128, 512).transpose(1, 0, 2)).astype(bf)
    g = 2 * np.pi * np.arange(512)[:, None] * np.arange(256)[None, :] / 512
    out["Gc"] = np.ascontiguousarray((np.cos(g) / 512).reshape(4, 128, 256).transpose(1, 0, 2)).astype(bf)
    out["Gs"] = np.ascontiguousarray((-np.sin(g) / 512).reshape(4, 128, 256).transpose(1, 0, 2)).astype(bf)
    return out


def a2_inputs(hyX_o, hyc, P, l, hh, HC, FC):
    bf = ml_dtypes.bfloat16
    ch = slice(hh * 128, (hh + 1) * 128)
    w3 = np.concatenate([P['filt_w3'][l][:, o * 512 + d * 256 + hh * 128:o * 512 + d * 256 + hh * 128 + 128] for o in range(2) for d in range(2)], 1)
    fv = np.stack([P['filt_freq'][l], P['filt_b1'][l], P['filt_b2'][l]], 1).astype(np.float32)
    m = {"hyX": np.asarray(hyX_o), "hyc": np.asarray(hyc),
         "zT": HC["zT"], "zTc": HC["zTc"], "w1": np.ascontiguousarray(P['filt_w1'][l]), "w2": np.ascontiguousarray(P['filt_w2'][l]),
         "fv": np.ascontiguousarray(fv), "w3": np.ascontiguousarray(w3),
         "win": np.ascontiguousarray(HC["win_full"][:, ch, :]).astype(bf), "winc": np.ascontiguousarray(HC["winc_full"][:, :, ch]),
         "hb": bc128(P['hyena_bias'][l][:, ch]), "Cc": HC["Cc"], "Sc": HC["Sc"], "Gc": HC["Gc"], "Gs": HC["Gs"]}
    m.update(FC)
    return m


def build_A4(c=None):
    c = c or Ctx()
    S = c.S
    pXd = c.din("pX", [64, 256, 64], BF16)
    pcd = c.din("pc", [256, 256], BF16)
    Ccd = c.din("Cc", [128, 2, 512], BF16)
    Scd = c.din("Sc", [128, 2, 512], BF16)
    fX = c.dout("fX", [128, 64, 64])
    fco = c.dout("fc", [256, 128])
    F = FFT(c, nk1=64)
    pXs = c.sb("pXs", [64, 256, 64], BF16)
    c.load("sync", pXs, pXd, "pXs")
    for pa in range(0, 64, 2):
        v1, k1 = F.fwd2(pXs[:, 0:128, :], ["pXs"], pa)
        v2, k2 = F.fwd2(pXs[:, 128:256, :], ["pXs"], pa)
        t, tk = c.rotbuf("f4t", [128, 2, 64], F32, 2)
        fo, fok = c.rotbuf("f4o", [128, 2, 64], F32, 3)
        S.op("scalar", lambda e, v2=v2, t=t: e.activation(out=t, in_=v2[:, :, 1, :], func=AF.Copy, scale=1.0 / 64.0), reads=[k2], writes=[tk])
        S.op("vector", lambda e, v1=v1, t=t, fo=fo: e.scalar_tensor_tensor(out=fo, in0=v1[:, :, 0, :], scalar=1.0 / 64.0, in1=t, op0=ALU.mult, op1=ALU.add),
             reads=[k1, tk], writes=[fok])
        c.store("sync", fX[:, pa:pa + 2, :], fo, fok)
    Cc = c.sb("Ccs", [128, 2, 512], BF16); Sc = c.sb("Scs", [128, 2, 512], BF16)
    pcs = c.sb("pcs", [128, 2, 256], BF16); np2 = c.sb("np2", [128, 2, 128], BF16)
    c.load("sync", Cc, Ccd, "Cc"); c.load("sync", Sc, Scd, "Sc")
    c.load("sync", pcs, pcd.rearrange("(t p) n -> p t n", p=128), "pcs")
    S.op("vector", lambda e: e.tensor_scalar(out=np2, in0=pcs[:, :, 128:256], scalar1=-1.0, scalar2=None, op0=ALU.mult), reads=["pcs"], writes=["np2"])
    for kt in range(2):
        ps, pk = c.psum()
        mm = []
        for tl in range(2):
            mm.append(lambda e, ps=ps, kt=kt, tl=tl: e.matmul(ps[:, 0:128], lhsT=Cc[:, tl, kt * 256:kt * 256 + 256:2], rhs=pcs[:, tl, 0:128], start=(tl == 0), stop=False))
            mm.append(lambda e, ps=ps, kt=kt, tl=tl: e.matmul(ps[:, 0:128], lhsT=Sc[:, tl, kt * 256:kt * 256 + 256:2], rhs=np2[:, tl, :], start=False, stop=(tl == 1)))
        S.op("tensor", mm, reads=["Cc", "Sc", "pcs", "np2"], writes=[pk])
        fo, fok = c.rotbuf("f4c", [128, 128], F32, 2)
        S.op("scalar", lambda e, ps=ps, fo=fo: e.activation(out=fo, in_=ps[:, 0:128], func=AF.Copy, scale=1.0 / 16.0), reads=[pk], writes=[fok])
        c.store("sync", fco[kt * 128:(kt + 1) * 128, :], fo, fok)
    return c


def fX_to_tokens(fx):
    a = np.asarray(fx).reshape(2, 64, 64, 64)
    return np.ascontiguousarray(a.transpose(1, 3, 2, 0).reshape(4096, 128))


def zX_to_tokens(zx):
    return np.ascontiguousarray(np.asarray(zx).transpose(0, 2, 1).reshape(4096, 128))


NT = TPC * 128


def build_B1(c=None):
    c = c or Ctx()
    S = c.S
    xd = c.din("x", [NT, 1024])
    mTd = c.din("mT", [1024, NT])
    wod = c.din("wo", [1024, 1024])
    g1d = c.din("g1", [128, 2, 1024])
    lngd = c.din("lng", [128, 1024]); lnbd = c.din("lnb", [128, 1024])
    sc2d = c.din("sc2", [128, 2, 1024]); sh2d = c.din("sh2", [128, 2, 1024])
    x1o = c.dout("x1", [NT, 1024]); h2o = c.dout("h2", [NT, 1024]); h2bo = c.dout("h2b", [NT, 1024], BF16)
    mT = c.sb("mTs", [128, 8, NT], BF16); wo = c.sb("wos", [128, 8, 1024], BF16)
    g1 = c.sb("g1s", [128, 2, 1024]); lng = c.sb("lngs", [128, 1024]); lnb = c.sb("lnbs", [128, 1024])
    sc2 = c.sb("sc2s", [128, 2, 1024]); sh2 = c.sb("sh2s", [128, 2, 1024])
    epsb = make_eps(c)
    for kc in range(8):
        for hh_ in range(2):
            S.dma("gpsimd", lambda e, kc=kc, hh_=hh_: e.dma_start(out=mT[:, kc, hh_ * (NT // 2):(hh_ + 1) * (NT // 2)],
                                                                  in_=mTd[kc * 128:(kc + 1) * 128, hh_ * (NT // 2):(hh_ + 1) * (NT // 2)]), writes=["mT"])
        S.dma("gpsimd", lambda e, kc=kc: e.dma_start(out=wo[:, kc, :], in_=wod[kc * 128:(kc + 1) * 128, :]), writes=["wo"])
    for (d, s, k) in ((g1d, g1, "g1"), (lngd, lng, "lng"), (lnbd, lnb, "lnb"), (sc2d, sc2, "sc2"), (sh2d, sh2, "sh2")):
        c.load("sync", s, d, k)
    S.op("vector", lambda e: e.tensor_scalar(out=sc2, in0=sc2, scalar1=1.0, scalar2=None, op0=ALU.add), reads=["sc2"], writes=["sc2"])
    for t in range(TPC):
        m = 1 if t == TPC - 1 else 0
        xt, xk = c.rotbuf("b1x", [128, 1024], F32, 2)
        c.load("sync", xt, xd[t * 128:(t + 1) * 128, :], xk)
        tmp, tmpk = c.rotbuf("b1tmp", [128, 1024], F32, 2)
        for hf in range(2):
            ps, pk = c.psum()
            S.op("tensor", [(lambda e, ps=ps, kc=kc, t=t, hf=hf: e.matmul(ps[:, 0:512], lhsT=mT[:, kc, t * 128:(t + 1) * 128], rhs=wo[:, kc, hf * 512:(hf + 1) * 512],
                                                                       start=(kc == 0), stop=(kc == 7))) for kc in range(8)], reads=["mT", "wo"], writes=[pk])
            S.op("vector", lambda e, ps=ps, tmp=tmp, hf=hf, m=m: e.tensor_tensor(out=tmp[:, hf * 512:(hf + 1) * 512], in0=ps[:, 0:512], in1=g1[:, m, hf * 512:(hf + 1) * 512], op=ALU.mult),
                 reads=[pk, "g1"], writes=[tmpk])
        pre, prek = c.rotbuf("b1pre", [128, 1024], F32, 2)
        S.op("vector", lambda e, xt=xt, tmp=tmp, pre=pre: e.scalar_tensor_tensor(out=pre, in0=xt, scalar=ALPHA, in1=tmp, op0=ALU.mult, op1=ALU.add), reads=[xk, tmpk], writes=[prek])
        n1, n1k = c.rotbuf("b1n1", [128, 1024], F32, 2)
        ln_tile(c, pre, prek, n1, n1k, epsb)
        x1, x1k = c.rotbuf("b1x1", [128, 1024], F32, 2)
        S.op("gpsimd", lambda e, n1=n1: e.tensor_tensor(out=n1, in0=n1, in1=lng, op=ALU.mult), reads=[n1k, "lng"], writes=[n1k])
        S.op("gpsimd", lambda e, n1=n1, x1=x1: e.tensor_tensor(out=x1, in0=n1, in1=lnb, op=ALU.add), reads=[n1k, "lnb"], writes=[x1k])
        c.store("sync", x1o[t * 128:(t + 1) * 128, :], x1, x1k)
        n2, n2k = c.rotbuf("b1n2", [128, 1024], F32, 2)
        ln_tile(c, x1, x1k, n2, n2k, epsb)
        h2, h2k = c.rotbuf("b1h2", [128, 1024], F32, 2)
        h2b, h2bk = c.rotbuf("b1h2b", [128, 1024], BF16, 2)
        S.op("vector", lambda e, n2=n2, m=m: e.tensor_tensor(out=n2, in0=n2, in1=sc2[:, m, :], op=ALU.mult), reads=[n2k, "sc2"], writes=[n2k])
        S.op("gpsimd", lambda e, n2=n2, h2=h2, m=m: e.tensor_tensor(out=h2, in0=n2, in1=sh2[:, m, :], op=ALU.add), reads=[n2k, "sh2"], writes=[h2k])
        S.op("scalar", lambda e, h2=h2, h2b=h2b: e.copy(out=h2b, in_=h2), reads=[h2k], writes=[h2bk])
        c.store("sync", h2o[t * 128:(t + 1) * 128, :], h2, h2k)
        c.store("sync", h2bo[t * 128:(t + 1) * 128, :], h2b, h2bk)
    return c


NEXP = 64


def build_B2(c=None, wsrc=None):
    c = c or Ctx()
    S = c.S
    hTd = c.din("hT", [1024, NT], BF16)
    hT32d = c.din("hT32", [1024, NT])
    x1d = c.din("x1", [NT, 1024])
    rwd = c.din("rw", [1024, 64]); rbd = c.din("rb", [128, 64])
    if wsrc is None:
        wgd = c.din("wg", [NEXP + 1, 1024, 256]); wud = c.din("wu", [NEXP + 1, 1024, 256]); wdd = c.din("wd", [NEXP + 1, 256, 1024])
        wsrc = {"wg": lambda ex: wgd[ex], "wu": lambda ex: wud[ex], "wd": lambda ex: wdd[ex]}
    g2d = c.din("g2", [128, 2, 1024]); lngd = c.din("lng", [128, 1024]); lnbd = c.din("lnb", [128, 1024])
    x2o = c.dout("x2", [NT, 1024])
    hT = c.sb("hTs", [128, 8, NT], BF16)
    rw = c.sb("rws", [128, 8, 64]); rb = c.sb("rbs", [128, 64])
    G = c.sb("G", [128, TPC, 64])
    yacc = c.sb("yacc", [128, TPC, 1024])
    aT = c.sb("aT", [128, 2, NT], BF16)
    g2 = c.sb("g2s", [128, 2, 1024]); lng = c.sb("lngs", [128, 1024]); lnb = c.sb("lnbs", [128, 1024])
    epsb = make_eps(c)
    for kc in range(8):
        c.load("sync", hT[:, kc, :], hTd[kc * 128:(kc + 1) * 128, :], "hT")
    c.load("sync", rw, rwd.rearrange("(k p) n -> p k n", p=128), "rw")
    for (d, s, k) in ((rbd, rb, "rb"), (g2d, g2, "g2"), (lngd, lng, "lng"), (lnbd, lnb, "lnb")):
        c.load("sync", s, d, k)
    h32v = hT32d.rearrange("(k p) n -> p k n", p=128)
    for t in range(TPC):
        h32, h32k = c.rotbuf("h32", [128, 8, 128], F32, 2)
        c.load("sync", h32, h32v[:, :, t * 128:(t + 1) * 128], h32k)
        ps, pk = c.psum()
        S.op("tensor", [(lambda e, ps=ps, kc=kc, h32=h32: e.matmul(ps[:, 0:64], lhsT=h32[:, kc, :], rhs=rw[:, kc, :], start=(kc == 0), stop=(kc == 7))) for kc in range(8)],
             reads=[h32k, "rw"], writes=[pk])
        s_, sk = c.rotbuf("r_s", [128, 64], F32, 2)
        sel, selk = c.rotbuf("r_sel", [128, 64], F32, 2)
        m8, m8k = c.rotbuf("r_m8", [128, 8, 8], F32, 2)
        gs, gsk = c.rotbuf("r_gs", [128, 8], F32, 2)
        g8, g8k = c.rotbuf("r_g8", [128, 8], F32, 2)
        gm, gmk = c.rotbuf("r_gm", [128, 8], F32, 2)
        pen, penk = c.rotbuf("r_pen", [128, 8], F32, 2)
        selm, selmk = c.rotbuf("r_selm", [128, 64], F32, 2)
        e8, e8k = c.rotbuf("r_e8", [128, 8], F32, 2)
        em, emk = c.rotbuf("r_em", [128, 64], F32, 2)
        den, denk = c.rotbuf("r_den", [128, 1], F32, 2)
        S.op("scalar", lambda e, ps=ps, s_=s_: e.activation(out=s_, in_=ps[:, 0:64], func=AF.Sigmoid), reads=[pk], writes=[sk])
        S.op("vector", lambda e, s_=s_, sel=sel: e.tensor_tensor(out=sel, in0=s_, in1=rb, op=ALU.add), reads=[sk, "rb"], writes=[selk])
        S.op("vector", [(lambda e, g=g, sel=sel, m8=m8: e.max(out=m8[:, g, :], in_=sel[:, g * 8:(g + 1) * 8])) for g in range(8)], reads=[selk], writes=[m8k])
        S.op("vector", lambda e, m8=m8, gs=gs: e.tensor_tensor(out=gs, in0=m8[:, :, 0], in1=m8[:, :, 1], op=ALU.add), reads=[m8k], writes=[gsk])
        S.op("vector", lambda e, gs=gs, g8=g8: e.max(out=g8, in_=gs), reads=[gsk], writes=[g8k])
        S.op("vector", lambda e, gs=gs, g8=g8, gm=gm: e.tensor_scalar(out=gm, in0=gs, scalar1=g8[:, 3:4], scalar2=None, op0=ALU.is_ge), reads=[gsk, g8k], writes=[gmk])
        S.op("vector", lambda e, gm=gm, pen=pen: e.tensor_scalar(out=pen, in0=gm, scalar1=4.0, scalar2=-4.0, op0=ALU.mult, op1=ALU.add), reads=[gmk], writes=[penk])
        S.op("vector", [(lambda e, g=g, sel=sel, selm=selm, gm=gm, pen=pen: e.tensor_scalar(out=selm[:, g * 8:(g + 1) * 8], in0=sel[:, g * 8:(g + 1) * 8], scalar1=gm[:, g:g + 1], scalar2=pen[:, g:g + 1],
                                                                                          op0=ALU.mult, op1=ALU.add)) for g in range(8)], reads=[selk, gmk, penk], writes=[selmk])
        S.op("vector", lambda e, selm=selm, e8=e8: e.max(out=e8, in_=selm), reads=[selmk], writes=[e8k])
        S.op("vector", lambda e, selm=selm, e8=e8, em=em: e.tensor_scalar(out=em, in0=selm, scalar1=e8[:, 7:8], scalar2=None, op0=ALU.is_ge), reads=[selmk, e8k], writes=[emk])
        S.op("vector", lambda e, em=em, s_=s_: e.tensor_tensor(out=em, in0=em, in1=s_, op=ALU.mult), reads=[emk, sk], writes=[emk])
        S.op("vector", lambda e, em=em, den=den: e.reduce_sum(out=den, in_=em, axis=AX.X), reads=[emk], writes=[denk])
        S.op("vector", lambda e, den=den: e.reciprocal(out=den, in_=den), reads=[denk], writes=[denk])
        S.op("vector", lambda e, em=em, den=den, t=t: e.tensor_scalar(out=G[:, t, :], in0=em, scalar1=den[:, 0:1], scalar2=2.5, op0=ALU.mult, op1=ALU.mult), reads=[emk, denk], writes=["G"])
    chunks = [(i * 512, 512) for i in range(NT // 512)] + ([(NT - NT % 512, NT % 512)] if NT % 512 else [])
    for ex in range(NEXP + 1):
        wg, wgk = c.rotbuf("wg", [128, 8, 256], BF16, 2)
        wu, wuk = c.rotbuf("wu", [128, 8, 256], BF16, 2)
        wd, wdk = c.rotbuf("wd", [128, 2, 1024], BF16, 2)
        S.dma("gpsimd", lambda e, wg=wg, ex=ex: e.dma_start(out=wg, in_=wsrc["wg"](ex).rearrange("(k p) n -> p k n", p=128)), reads=["wall"], writes=[wgk])
        S.dma("gpsimd", lambda e, wu=wu, ex=ex: e.dma_start(out=wu, in_=wsrc["wu"](ex).rearrange("(k p) n -> p k n", p=128)), reads=["wall"], writes=[wuk])
        S.dma("gpsimd", lambda e, wd=wd, ex=ex: e.dma_start(out=wd, in_=wsrc["wd"](ex).rearrange("(k p) n -> p k n", p=128)), reads=["wall"], writes=[wdk])
        for (c0, n) in chunks:
            for mh in range(2):
                pg, pgk = c.psum()
                S.op("tensor", [(lambda e, pg=pg, kc=kc, mh=mh, c0=c0, n=n, wg=wg: e.matmul(pg[:, 0:n], lhsT=wg[:, kc, mh * 128:(mh + 1) * 128], rhs=hT[:, kc, c0:c0 + n], start=(kc == 0), stop=(kc == 7))) for kc in range(8)],
                     reads=["hT", wgk], writes=[pgk])
                pu, puk = c.psum()
                S.op("tensor", [(lambda e, pu=pu, kc=kc, mh=mh, c0=c0, n=n, wu=wu: e.matmul(pu[:, 0:n], lhsT=wu[:, kc, mh * 128:(mh + 1) * 128], rhs=hT[:, kc, c0:c0 + n], start=(kc == 0), stop=(kc == 7))) for kc in range(8)],
                     reads=["hT", wuk], writes=[puk])
                sg, sgk = c.rotbuf("sg", [128, 512], F32, 2)
                S.op("scalar", lambda e, pg=pg, sg=sg, n=n: e.activation(out=sg[:, 0:n], in_=pg[:, 0:n], func=AF.Silu), reads=[pgk], writes=[sgk])
                S.op("vector", lambda e, pu=pu, sg=sg, n=n, mh=mh, c0=c0: e.tensor_tensor(out=aT[:, mh, c0:c0 + n], in0=pu[:, 0:n], in1=sg[:, 0:n], op=ALU.mult), reads=[puk, sgk], writes=["aT"])
        for t in range(TPC):
            for hf in range(2):
                py, pyk = c.psum()
                S.op("tensor", [(lambda e, py=py, mh=mh, t=t, hf=hf, wd=wd: e.matmul(py[:, 0:512], lhsT=aT[:, mh, t * 128:(t + 1) * 128], rhs=wd[:, mh, hf * 512:(hf + 1) * 512], start=(mh == 0), stop=(mh == 1))) for mh in range(2)],
                     reads=["aT", wdk], writes=[pyk])
                ya = yacc[:, t, hf * 512:(hf + 1) * 512]
                yk = ("yacc", t, hf)
                if ex == 0:
                    S.op("vector", lambda e, py=py, ya=ya, t=t, ex=ex: e.tensor_scalar(out=ya, in0=py[:, 0:512], scalar1=G[:, t, ex:ex + 1], scalar2=None, op0=ALU.mult), reads=[pyk, "G"], writes=[yk])
                elif ex < NEXP:
                    S.op("vector", lambda e, py=py, ya=ya, t=t, ex=ex: e.scalar_tensor_tensor(out=ya, in0=py[:, 0:512], scalar=G[:, t, ex:ex + 1], in1=ya, op0=ALU.mult, op1=ALU.add), reads=[pyk, "G", yk], writes=[yk])
                else:
                    S.op("vector", lambda e, py=py, ya=ya: e.tensor_tensor(out=ya, in0=py[:, 0:512], in1=ya, op=ALU.add), reads=[pyk, yk], writes=[yk])
    for t in range(TPC):
        m = 1 if t == TPC - 1 else 0
        xt, xk = c.rotbuf("b2x", [128, 1024], F32, 1)
        c.load("sync", xt, x1d[t * 128:(t + 1) * 128, :], xk)
        yt = yacc[:, t, :]
        yks = [("yacc", t, 0), ("yacc", t, 1)]
        S.op("gpsimd", lambda e, yt=yt, m=m: e.tensor_tensor(out=yt, in0=yt, in1=g2[:, m, :], op=ALU.mult), reads=yks + ["g2"], writes=yks)
        pre, prek = c.rotbuf("b2pre", [128, 1024], F32, 1)
        S.op("vector", lambda e, xt=xt, yt=yt, pre=pre: e.scalar_tensor_tensor(out=pre, in0=xt, scalar=ALPHA, in1=yt, op0=ALU.mult, op1=ALU.add), reads=[xk] + yks, writes=[prek])
        n1, n1k = c.rotbuf("b2n", [128, 1024], F32, 1)
        ln_tile(c, pre, prek, n1, n1k, epsb)
        xo, xok = c.rotbuf("b2o", [128, 1024], F32, 2)
        S.op("gpsimd", lambda e, n1=n1: e.tensor_tensor(out=n1, in0=n1, in1=lng, op=ALU.mult), reads=[n1k, "lng"], writes=[n1k])
        S.op("gpsimd", lambda e, n1=n1, xo=xo: e.tensor_tensor(out=xo, in0=n1, in1=lnb, op=ALU.add), reads=[n1k, "lnb"], writes=[xok])
        c.store("sync", x2o[t * 128:(t + 1) * 128, :], xo, xok)
    return c


_PROGS = {}


def _prog(name, builder):
    if name not in _PROGS:
        p = builder()
        p.S.finish()
        _PROGS[name] = p
    return _PROGS[name]


def _run(p, maps):
    return run_bass_kernel_spmd(p.nc, maps, core_ids=list(range(8))).results


def _shard_rows(main, ctxa, r):
    b, hf = r // 2, r % 2
    return np.ascontiguousarray(np.concatenate([main[b, hf * 2048:(hf + 1) * 2048], ctxa[b, hf * 128:(hf + 1) * 128]], 0))


def _unshard_rows(res, key):
    main = np.stack([np.concatenate([np.asarray(res[2 * b + hf][key])[0:2048] for hf in range(2)], 0) for b in range(4)], 0)
    ctxa = np.stack([np.concatenate([np.asarray(res[2 * b + hf][key])[2048:2176] for hf in range(2)], 0) for b in range(4)], 0)
    return main, ctxa


def kernel_unfused32(**inp):
    D = 1024
    x = np.asarray(inp["x"], np.float32)
    xc = np.asarray(inp["ctx"], np.float32)
    cvec, c_ctx = inp["c"], inp["c_ctx"]
    pM = _prog("M", build_M); pA = _prog("A1a", build_A1a); pB = _prog("A1b", build_A1b)
    p2 = _prog("A2", build_A2); p3 = _prog("A3", build_A3); p4 = _prog("A4", build_A4)
    pB1 = _prog("B1", build_B1); pB2 = _prog("B2", build_B2)
    ropec, ropes = rope_tables(); c64 = dft64_consts(); HC = hyena_consts(); FC = fft_consts()
    for l in range(4):
        mod = run_M(pM, cvec, c_ctx, inp["w_mod"][l], inp["b_mod"][l])
        maps = []
        for r in range(8):
            b = r // 2
            maps.append({"x": _shard_rows(x, xc, r), "sc": bc128(np.stack([mod[b, D:2 * D], mod[4, D:2 * D]], 0)),
                         "sh": bc128(np.stack([mod[b, 0:D], mod[4, 0:D]], 0))})
        h_main, h_ctx = _unshard_rows(_run(pA, maps), "h")
        hTs = [pad_hT(h_main[b], h_ctx[b]) for b in range(4)]
        maps = [a1b_inputs(hTs[r // 2], inp["w_in"][l], inp["short_conv_w"][l], inp["fnet_w"][l], r % 2, ropec, ropes, c64) for r in range(8)]
        rB = _run(pB, maps)
        maps = [a2_inputs(np.asarray(rB[r]["hyX"]), np.asarray(rB[r]["hyc"]), inp, l, r % 2, HC, FC) for r in range(8)]
        r2 = _run(p2, maps)
        li = lam_init_of(l)
        lamv = bc128(np.stack([inp["lam_q1"][l], inp["lam_k1"][l], inp["lam_q2"][l], inp["lam_k2"][l]], 0))
        maps = [{"qT": np.asarray(rB[r]["qT"]), "kT": np.asarray(rB[r]["kT"]), "V": np.asarray(rB[r]["V"]), "lamv": lamv,
                 "lami": bc128(np.array([li, 1 - li], np.float32)), "subw": bc128(inp["subln_w"][l])} for r in range(8)]
        r3 = _run(p3, maps)
        maps = [{"pX": np.asarray(rB[r]["pX"]), "pc": np.asarray(rB[r]["pc"]), "Cc": HC["Cc"], "Sc": HC["Sc"],
                 "D1": FC["D1"], "TW2": FC["TW2"], "D2": FC["D2"]} for r in range(8)]
        r4 = _run(p4, maps)
        del rB
        mixed = np.zeros((4, 4096, D), np.float32); mixed_c = np.zeros((4, 256, D), np.float32)
        for r in range(8):
            b, hh = r // 2, r % 2
            mixed[b, :, hh * 128:(hh + 1) * 128] = zX_to_tokens(r2[r]["zX"])
            mixed_c[b, :, hh * 128:(hh + 1) * 128] = np.asarray(r2[r]["zc"])
            att = np.asarray(r3[r]["att"])
            mixed[b, :, 256 + hh * 256:256 + (hh + 1) * 256] = att[0:4096]
            mixed_c[b, :, 256 + hh * 256:256 + (hh + 1) * 256] = att[4096:4352]
            mixed[b, :, 768 + hh * 128:768 + (hh + 1) * 128] = fX_to_tokens(r4[r]["fX"])
            mixed_c[b, :, 768 + hh * 128:768 + (hh + 1) * 128] = np.asarray(r4[r]["fc"])
        del r2, r3, r4
        maps = []
        for r in range(8):
            b = r // 2
            maps.append({"x": _shard_rows(x, xc, r), "mT": np.ascontiguousarray(_shard_rows(mixed, mixed_c, r).T), "wo": np.ascontiguousarray(inp["w_out"][l]),
                         "g1": bc128(np.stack([mod[b, 2 * D:3 * D], mod[4, 2 * D:3 * D]], 0)), "lng": bc128(inp["ln1_g"][l]), "lnb": bc128(inp["ln1_b"][l]),
                         "sc2": bc128(np.stack([mod[b, 4 * D:5 * D], mod[4, 4 * D:5 * D]], 0)), "sh2": bc128(np.stack([mod[b, 3 * D:4 * D], mod[4, 3 * D:4 * D]], 0))})
        rb1 = _run(pB1, maps)
        wg = np.concatenate([inp["exp_w_gate"][l], inp["sh_w_gate"][l][None]], 0)
        wu = np.concatenate([inp["exp_w_up"][l], inp["sh_w_up"][l][None]], 0)
        wd = np.concatenate([inp["exp_w_down"][l], inp["sh_w_down"][l][None]], 0)
        maps = []
        for r in range(8):
            b = r // 2
            maps.append({"hT": np.ascontiguousarray(np.asarray(rb1[r]["h2b"]).T), "hT32": np.ascontiguousarray(np.asarray(rb1[r]["h2"]).T),
                         "x1": np.asarray(rb1[r]["x1"]), "rw": np.ascontiguousarray(inp["router_w"][l]), "rb": bc128(inp["router_b"][l]),
                         "wg": wg, "wu": wu, "wd": wd, "g2": bc128(np.stack([mod[b, 5 * D:6 * D], mod[4, 5 * D:6 * D]], 0)),
                         "lng": bc128(inp["ln2_g"][l]), "lnb": bc128(inp["ln2_b"][l])})
        del rb1
        rb2 = _run(pB2, maps)
        x, xc = _unshard_rows(rb2, "x2")
        x = np.ascontiguousarray(x, np.float32); xc = np.ascontiguousarray(xc, np.float32)
        del rb2, maps, wg, wu, wd
    return x


def phase_ln_to_hT(c, xfull, scd, shd, identbd):
    S = c.S
    hTs = c.sb("hTs", [128, 8, 4356], BF16)
    m0 = c.mark()
    sc = c.sb("a1sc", [128, 2, 1024]); sh = c.sb("a1sh", [128, 2, 1024]); identb = c.sb("identb", [128, 128], BF16)
    epsb = make_eps(c)
    c.load("sync", sc, scd, "sc"); c.load("sync", sh, shd, "sh"); c.load("sync", identb, identbd, "identb")
    S.op("vector", lambda e: e.tensor_scalar(out=sc, in0=sc, scalar1=1.0, scalar2=None, op0=ALU.add), reads=["sc"], writes=["sc"])
    for col in (0, 4097, 4098, 4355):
        S.op("gpsimd", lambda e, col=col: e.memset(hTs[:, :, col:col + 1], 0.0), writes=["hT"])
    pT = c.psb
    for t in range(34):
        m = 1 if t >= 32 else 0
        col0 = 1 + 128 * t if t < 32 else 4099 + 128 * (t - 32)
        xt, xk = c.rotbuf("xt", [128, 1024], F32, 2)
        xn, nk = c.rotbuf("xn", [128, 1024], F32, 2)
        t2, tk = c.rotbuf("t2", [128, 1024], F32, 2)
        ho, hk = c.rotbuf("ho", [128, 1024], BF16, 2)
        row0 = (t // 16) * 2176 + (t % 16) * 128 if t < 32 else (t - 32) * 2176 + 2048
        c.load("sync", xt, xfull[row0:row0 + 128, :], xk)
        ln_tile(c, xt, xk, xn, nk, epsb)
        S.op("vector", lambda e, xn=xn, t2=t2, m=m: e.tensor_tensor(out=t2, in0=xn, in1=sc[:, m, :], op=ALU.mult), reads=[nk, "sc"], writes=[tk])
        S.op("gpsimd", lambda e, t2=t2, ho=ho, m=m: e.tensor_tensor(out=ho, in0=t2, in1=sh[:, m, :], op=ALU.add), reads=[tk, "sh"], writes=[hk])
        S.op("tensor", [(lambda e, ho=ho, kc=kc: e.transpose(out=pT[:, kc * 128:(kc + 1) * 128], in_=ho[:, kc * 128:(kc + 1) * 128], identity=identb)) for kc in range(8)],
             reads=[hk, "identb"], writes=["psb"])
        if t % 2:
            S.op("scalar", lambda e, col0=col0: e.copy(out=hTs[:, :, col0:col0 + 128], in_=pT.rearrange("p (k t) -> p k t", k=8)), reads=["psb"], writes=["hT"])
        else:
            S.op("vector", lambda e, col0=col0: e.tensor_copy(out=hTs[:, :, col0:col0 + 128], in_=pT.rearrange("p (k t) -> p k t", k=8)), reads=["psb"], writes=["hT"])
    c.release(m0)
    return hTs


def build_FA(c=None, xfull=None, mloc=None):
    c = c or Ctx()
    S = c.S
    if xfull is None:
        xfull = c.din("xfull", [4352, 1024])
    scd = c.din("sc1", [128, 2, 1024]); shd = c.din("sh1", [128, 2, 1024])
    identbd = c.din("identb", [128, 128], BF16); identfd = c.din("identf", [128, 128])
    if mloc is None:
        mloc = c.dout("mloc", [512, 4352])
    specs = {"hyX": ([64, 384, 64], BF16), "pX": ([64, 256, 64], BF16), "hyc": ([256, 384], BF16), "pc": ([256, 256], BF16), "qT": ([256, 4352], BF16),
             "kT": ([256, 4352], BF16), "V": ([4352, 256], BF16), "zX": ([64, 128, 64], F32), "zc": ([256, 128], F32), "att": ([4352, 256], F32),
             "fX": ([128, 64, 64], F32), "fc": ([256, 128], F32)}
    sc_ = {}
    for k, (shp, dt) in specs.items():
        if k not in c.override:
            c.override[k] = c.scratch("s_" + k, shp, dt)
        sc_[k] = c.override[k]
    base = c.mark()
    hTs = phase_ln_to_hT(c, xfull, scd, shd, identbd)
    build_A1b(c, hTs)
    c.release(base)
    build_A2(c)
    c.release(base)
    build_A3(c)
    c.release(base)
    build_A4(c)
    c.release(base)
    dst = mloc[0:128, 0:4096].rearrange("c (n1 n2) -> n1 c n2", n2=64)
    for g in range(8):
        S.dma("sync", lambda e, g=g: e.dma_start(out=dst[g * 8:(g + 1) * 8], in_=sc_["zX"][g * 8:(g + 1) * 8]), writes=["mloc"], is_output=True)
    dst2 = mloc[384:512, 0:4096].rearrange("(p c2) (k2 k) -> c2 k2 p k", c2=2, k=64)
    src2 = sc_["fX"].rearrange("(c2 k2) p k -> c2 k2 p k", c2=2)
    for c2 in range(2):
        for g in range(8):
            S.dma("sync", lambda e, c2=c2, g=g: e.dma_start(out=dst2[c2, g * 8:(g + 1) * 8], in_=src2[c2, g * 8:(g + 1) * 8]), writes=["mloc"], is_output=True)
    identf = c.sb("identf", [128, 128])
    c.load("sync", identf, identfd, "identf")
    jobs = [(sc_["att"], tt, 256, 128, tt * 128) for tt in range(34)]
    jobs += [(sc_["zc"], tl, 128, 0, 4096 + tl * 128) for tl in range(2)] + [(sc_["fc"], tl, 128, 384, 4096 + tl * 128) for tl in range(2)]
    for (src, tt, ncol, row0, col0) in jobs:
        nb = ncol // 128
        ti, tik = c.rotbuf("tr_in", [128, 256], F32, 3)
        to, tok = c.rotbuf("tr_out", [128, 2, 128], F32, 3)
        c.load("sync", ti[:, 0:ncol], src[tt * 128:(tt + 1) * 128, :], tik)
        ps, pk = c.psum()
        S.op("tensor", [(lambda e, ps=ps, ti=ti, bb=bb: e.transpose(out=ps[:, bb * 128:(bb + 1) * 128], in_=ti[:, bb * 128:(bb + 1) * 128], identity=identf)) for bb in range(nb)],
             reads=[tik, "identf"], writes=[pk])
        S.op("vector", lambda e, ps=ps, to=to, nb=nb: e.tensor_copy(out=to[:, 0:nb, :], in_=ps[:, 0:nb * 128].rearrange("p (b t) -> p b t", b=nb)), reads=[pk], writes=[tok])
        S.dma("sync", lambda e, to=to, nb=nb, row0=row0, col0=col0: e.dma_start(
            out=mloc[row0:row0 + nb * 128, col0:col0 + 128].rearrange("(b c) t -> c b t", b=nb), in_=to[:, 0:nb, :]), reads=[tok], writes=["mloc"], is_output=True)
    c.release(base)
    return c


MIX_PERM = np.concatenate([np.arange(0, 128), np.arange(256, 512), np.arange(768, 896), np.arange(128, 256), np.arange(512, 768), np.arange(896, 1024)])


def fa_inputs(xfull, mod, b, hh, inp, l, CONST):
    D = 1024
    m = {"xfull": xfull, "sc1": bc128(np.stack([mod[b, D:2 * D], mod[4, D:2 * D]], 0)), "sh1": bc128(np.stack([mod[b, 0:D], mod[4, 0:D]], 0)),
         "identb": CONST["identb"], "identf": CONST["identf"]}
    a1 = a1b_inputs(None, inp["w_in"][l], inp["short_conv_w"][l], inp["fnet_w"][l], hh, CONST["ropec"], CONST["ropes"], CONST["c64"])
    del a1["hT"]
    m.update(a1)
    a2 = a2_inputs(None, None, inp, l, hh, CONST["HC"], CONST["FC"])
    del a2["hyX"], a2["hyc"]
    m.update(a2)
    li = lam_init_of(l)
    m.update({"lamv": bc128(np.stack([inp["lam_q1"][l], inp["lam_k1"][l], inp["lam_q2"][l], inp["lam_k2"][l]], 0)),
              "lami": bc128(np.array([li, 1 - li], np.float32)), "subw": bc128(inp["subln_w"][l])})
    return m


def make_consts():
    ropec, ropes = rope_tables()
    return {"ropec": ropec, "ropes": ropes, "c64": dft64_consts(), "HC": hyena_consts(), "FC": fft_consts(),
            "identb": np.eye(128, dtype=np.float32).astype(ml_dtypes.bfloat16), "identf": np.eye(128, dtype=np.float32)}


PAIRS = [[0, 1], [2, 3], [4, 5], [6, 7]]
ALL8 = [list(range(8))]
MOD_CHUNK = {"sh1": 0, "sc1": 1, "g1": 2, "sh2": 3, "sc2": 4, "g2": 5}


def build_MEGA(nl=4):
    c = Ctx()
    S = c.S
    nc = c.nc
    x0full = c.din("x0full", [4352, 1024])
    x0loc = c.din("x0loc", [NT, 1024])
    hmaskd = c.din("hmask", [128, 2])
    seld = c.din("sel", [5, 2, 128])
    ccTd = c.din("ccT", [128, 8, 5])
    wmd = c.din("wm", [nl, 1024, 6144])
    bmd = c.din("bm", [nl, 5, 6144])
    yout = c.dout("y", [NT, 1024])
    shared_names = [("c64", [64, 2, 64], F32), ("ropec", [128, 4352], F32), ("ropes", [128, 4352], F32), ("identb", [128, 128], BF16), ("identf", [128, 128], F32),
                    ("zT", [33, 4096], F32), ("zTc", [33, 256], F32), ("win", [64, 128, 64], BF16), ("winc", [128, 2, 128], F32),
                    ("Cc", [128, 2, 512], BF16), ("Sc", [128, 2, 512], BF16), ("Gc", [128, 4, 256], BF16), ("Gs", [128, 4, 256], BF16),
                    ("D1", [64, 2, 128], BF16), ("TW2", [128, 2, 2, 128], F32), ("D2", [128, 3, 128], BF16), ("TWI", [128, 2, 2, 128], F32), ("D1I", [128, 2, 64], BF16)]
    for (nm, shp, dt) in shared_names:
        c.override[nm] = c.din(nm, shp, dt)
    sc_ = {"hyX": c.scratch("s_hyX", [64, 384, 64], BF16), "pX": c.scratch("s_pX", [64, 256, 64], BF16), "hyc": c.scratch("s_hyc", [256, 384], BF16),
           "pc": c.scratch("s_pc", [256, 256], BF16), "qT": c.scratch("s_qT", [256, 4352], BF16), "kT": c.scratch("s_kT", [256, 4352], BF16),
           "V": c.scratch("s_V", [4352, 256], BF16), "zX": c.scratch("s_zX", [64, 128, 64]), "zc": c.scratch("s_zc", [256, 128]),
           "att": c.scratch("s_att", [4352, 256]), "fX": c.scratch("s_fX", [128, 64, 64]), "fc": c.scratch("s_fc", [256, 128]),
           "x1": c.scratch("s_x1", [NT, 1024]), "h2": c.scratch("s_h2", [NT, 1024]), "h2b": c.scratch("s_h2b", [NT, 1024], BF16),
           "hT": c.scratch("s_hT", [1024, NT], BF16), "hT32": c.scratch("s_hT32", [1024, NT])}
    c.override.update(sc_)
    mloc = c.scratch("s_mloc", [512, 4352])
    Cb = c.scratch("s_Cb", [2 * 1024, NT])
    mrs = c.scratch("s_mrs", [1024, NT])
    x2loc = c.scratch("s_x2loc", [NT, 1024])
    xfulln = c.scratch("s_xfull", [2 * NT, 1024])
    modbc = {nm: c.scratch("s_bc_" + nm, [128, 2, 1024]) for nm in MOD_CHUNK}
    wall = {}
    for l in range(nl):
        for (nm, rows, cols) in (("wg", 1024, 256), ("wu", 1024, 256), ("wd", 256, 1024)):
            wall[(l, nm)] = c.din(f"L{l}_{nm}_all", [64 * rows, cols]).rearrange("(e r) n -> e r n", e=64)
    ccs = c.sb("ccs", [128, 8, 5]); scs = c.sb("scs", [128, 8, 5])
    c.load("sync", ccs, ccTd, "ccs")
    S.op("scalar", lambda e: e.activation(out=scs, in_=ccs, func=AF.Silu), reads=["ccs"], writes=["scs"])
    S.barrier()
    base = c.mark()

    for l in range(nl):
        last = (l == nl - 1)
        sel = c.sb("sel", [5, 2, 128]); rows5 = c.sb("rows5", [5, 6, 1024])
        c.load("sync", sel, seld, "sel")
        r5v = rows5.rearrange("p a b -> p (a b)")
        for q in range(8):
            wsb, wk = c.rotbuf("wmsb", [128, 8, 768], F32, 2)
            bmb, bk = c.rotbuf("bmsb", [5, 768], F32, 2)
            c.load("sync", wsb, wmd[l, :, q * 768:(q + 1) * 768].rearrange("(k p) n -> p k n", p=128), wk)
            c.load("sync", bmb, bmd[l, :, q * 768:(q + 1) * 768], bk)
            for h in range(2):
                ps, pk = c.psum()
                S.op("tensor", [(lambda e, kc=kc, ps=ps, h=h, wsb=wsb: e.matmul(ps[0:5, 0:384], lhsT=scs[:, kc, :], rhs=wsb[:, kc, h * 384:(h + 1) * 384], start=(kc == 0), stop=(kc == 7))) for kc in range(8)],
                     reads=[wk], writes=[pk])
                S.op("vector", lambda e, ps=ps, h=h, q=q, bmb=bmb: e.tensor_tensor(out=r5v[:, q * 768 + h * 384:q * 768 + (h + 1) * 384], in0=ps[0:5, 0:384], in1=bmb[:, h * 384:(h + 1) * 384], op=ALU.add),
                     reads=[pk, bk], writes=["rows5"])
        for nm, j in MOD_CHUNK.items():
            bct, bck = c.rotbuf("bct", [128, 2, 1024], F32, 2)
            for m in range(2):
                for h in range(2):
                    ps, pk = c.psum()
                    S.op("tensor", lambda e, ps=ps, m=m, h=h, j=j: e.matmul(ps[:, 0:512], lhsT=sel[:, m, :], rhs=rows5[:, j, h * 512:(h + 1) * 512], start=True, stop=True), reads=["sel", "rows5"], writes=[pk])
                    S.op("vector", lambda e, ps=ps, m=m, h=h, bct=bct: e.tensor_copy(out=bct[:, m, h * 512:(h + 1) * 512], in_=ps[:, 0:512]), reads=[pk], writes=[bck])
            S.dma("sync", lambda e, bct=bct, nm=nm: e.dma_start(out=modbc[nm], in_=bct), reads=[bck], writes=["modbc"])
        c.release(base)
        c.prefix = f"L{l}_"
        c.override.update({"sc1": modbc["sc1"], "sh1": modbc["sh1"]})
        build_FA(c, xfull=(x0full if l == 0 else xfulln), mloc=mloc)
        hm = c.sb("hm", [128, 2]); c.load("sync", hm, hmaskd, "hm")
        for rb in range(4):
            blk, bk = c.rotbuf("mblk", [128, 4352], F32, 2)
            c.load("sync", blk, mloc[rb * 128:(rb + 1) * 128, :], bk)
            for hsel in range(2):
                mb, mbk = c.rotbuf("mmask", [128, 4352], F32, 2)
                S.op("vector" if hsel == 0 else "gpsimd", lambda e, blk=blk, mb=mb, hsel=hsel: e.tensor_scalar(out=mb, in0=blk, scalar1=hm[:, hsel:hsel + 1], scalar2=None, op0=ALU.mult), reads=[bk, "hm"], writes=[mbk])
                for d in range(2):
                    r0 = d * 1024 + hsel * 512 + rb * 128
                    S.dma("sync", lambda e, mb=mb, r0=r0, d=d: e.dma_start(out=Cb[r0:r0 + 128, 0:2048], in_=mb[:, d * 2048:(d + 1) * 2048]), reads=[mbk], writes=["Cb"])
                    S.dma("sync", lambda e, mb=mb, r0=r0, d=d: e.dma_start(out=Cb[r0:r0 + 128, 2048:2176], in_=mb[:, 4096 + d * 128:4096 + (d + 1) * 128]), reads=[mbk], writes=["Cb"])
        c.release(base)
        S.collective(lambda e: e.collective_compute("ReduceScatter", ALU.add, replica_groups=PAIRS, ins=[Cb.opt()], outs=[mrs.opt()]), reads=["Cb"], writes=["mrs"])
        c.release(base)
        c.prefix = f"L{l}_B1_"
        c.override.update({"x": (x0loc if l == 0 else x2loc), "mT": mrs, "g1": modbc["g1"], "sc2": modbc["sc2"], "sh2": modbc["sh2"]})
        build_B1(c)
        c.release(base)
        identb = c.sb("identb", [128, 128], BF16); identf = c.sb("identf", [128, 128])
        c.load("sync", identb, c.override["identb"], "identb"); c.load("sync", identf, c.override["identf"], "identf")
        hTv = sc_["hT"].rearrange("(k p) n -> p k n", p=128); hT32v = sc_["hT32"].rearrange("(k p) n -> p k n", p=128)
        for t in range(TPC):
            tb, tbk = c.rotbuf("tb_in", [128, 1024], BF16, 2)
            tf, tfk = c.rotbuf("tf_in", [128, 1024], F32, 2)
            ob, obk = c.rotbuf("tb_out", [128, 8, 128], BF16, 2)
            of, ofk = c.rotbuf("tf_out", [128, 8, 128], F32, 2)
            c.load("sync", tb, sc_["h2b"][t * 128:(t + 1) * 128, :], tbk)
            c.load("sync", tf, sc_["h2"][t * 128:(t + 1) * 128, :], tfk)
            S.op("tensor", [(lambda e, tb=tb, kc=kc: e.transpose(out=c.psb[:, kc * 128:(kc + 1) * 128], in_=tb[:, kc * 128:(kc + 1) * 128], identity=identb)) for kc in range(8)],
                 reads=[tbk, "identb"], writes=["psb"])
            S.op("scalar", lambda e, ob=ob: e.copy(out=ob, in_=c.psb.rearrange("p (k t) -> p k t", k=8)), reads=["psb"], writes=[obk])
            S.dma("sync", lambda e, ob=ob, t=t: e.dma_start(out=hTv[:, :, t * 128:(t + 1) * 128], in_=ob), reads=[obk], writes=["hTd"])
            for h in range(2):
                ps, pk = c.psum()
                S.op("tensor", [(lambda e, tf=tf, ps=ps, kk=kk, h=h: e.transpose(out=ps[:, kk * 128:(kk + 1) * 128], in_=tf[:, (h * 4 + kk) * 128:(h * 4 + kk + 1) * 128], identity=identf)) for kk in range(4)],
                     reads=[tfk, "identf"], writes=[pk])
                S.op("vector", lambda e, of=of, ps=ps, h=h: e.tensor_copy(out=of[:, h * 4:(h + 1) * 4, :], in_=ps[:, 0:512].rearrange("p (k t) -> p k t", k=4)), reads=[pk], writes=[ofk])
            S.dma("sync", lambda e, of=of, t=t: e.dma_start(out=hT32v[:, :, t * 128:(t + 1) * 128], in_=of), reads=[ofk], writes=["hT32d"])
        c.release(base)
        c.prefix = f"L{l}_B2_"
        swg = c.din("swg", [1024, 256]); swu = c.din("swu", [1024, 256]); swd = c.din("swd", [256, 1024])
        wsrc = {"wg": (lambda ex, l=l, swg=swg: wall[(l, "wg")][ex] if ex < NEXP else swg),
                "wu": (lambda ex, l=l, swu=swu: wall[(l, "wu")][ex] if ex < NEXP else swu),
                "wd": (lambda ex, l=l, swd=swd: wall[(l, "wd")][ex] if ex < NEXP else swd)}
        c.override.update({"g2": modbc["g2"], "x2": (yout if last else x2loc)})
        build_B2(c, wsrc)
        c.release(base)
        if not last:
            S.collective(lambda e: e.collective_compute("AllGather", ALU.bypass, replica_groups=PAIRS, ins=[x2loc.opt()], outs=[xfulln.opt()]), reads=[], writes=["xfulln"])
            c.release(base)
    c.prefix = ""
    return c


def mega_inputs(inp, r, CONST, nl=4, loff=0):
    b, hh = r // 2, r % 2
    x, ctx = inp["x"], inp["ctx"]
    m = {}
    m["x0full"] = np.ascontiguousarray(np.concatenate([x[b, 0:2048], ctx[b, 0:128], x[b, 2048:4096], ctx[b, 128:256]], 0))
    m["x0loc"] = _shard_rows(x, ctx, r)
    m["hmask"] = bc128(np.array([1.0, 0.0] if hh == 0 else [0.0, 1.0], np.float32))
    sel = np.zeros((5, 2, 128), np.float32); sel[b, 0, :] = 1.0; sel[4, 1, :] = 1.0
    m["sel"] = sel
    m["ccT"] = silu_layout_cc(inp["c"], inp["c_ctx"])
    m["wm"] = np.ascontiguousarray(inp["w_mod"][loff:loff + nl])
    m["bm"] = np.ascontiguousarray(np.broadcast_to(inp["b_mod"][loff:loff + nl, None, :], (nl, 5, 6144)))
    HC, FC = CONST["HC"], CONST["FC"]
    ch = slice(hh * 128, (hh + 1) * 128)
    m.update({"c64": CONST["c64"], "ropec": CONST["ropec"], "ropes": CONST["ropes"], "identb": CONST["identb"], "identf": CONST["identf"],
              "zT": HC["zT"], "zTc": HC["zTc"], "win": np.ascontiguousarray(HC["win_full"][:, ch, :]).astype(ml_dtypes.bfloat16),
              "winc": np.ascontiguousarray(HC["winc_full"][:, :, ch]), "Cc": HC["Cc"], "Sc": HC["Sc"], "Gc": HC["Gc"], "Gs": HC["Gs"]})
    m.update(FC)
    for ll in range(nl):
        P = f"L{ll}_"
        l = ll + loff
        a1 = a1b_inputs(None, inp["w_in"][l], inp["short_conv_w"][l], inp["fnet_w"][l], hh, None, None, None)
        for k in ("w", "scw", "wfT", "fw"):
            m[P + k] = a1[k]
        a2 = a2_inputs(None, None, inp, l, hh, HC, {})
        for k in ("w1", "w2", "fv", "w3", "hb"):
            m[P + k] = a2[k]
        li = lam_init_of(l)
        m[P + "lamv"] = bc128(np.stack([inp["lam_q1"][l], inp["lam_k1"][l], inp["lam_q2"][l], inp["lam_k2"][l]], 0))
        m[P + "lami"] = bc128(np.array([li, 1 - li], np.float32))
        m[P + "subw"] = bc128(inp["subln_w"][l])
        m[P + "B1_wo"] = np.ascontiguousarray(inp["w_out"][l][MIX_PERM, :])
        m[P + "B1_lng"] = bc128(inp["ln1_g"][l]); m[P + "B1_lnb"] = bc128(inp["ln1_b"][l])
        m[P + "B2_rw"] = np.ascontiguousarray(inp["router_w"][l]); m[P + "B2_rb"] = bc128(inp["router_b"][l])
        m[P + "B2_lng"] = bc128(inp["ln2_g"][l]); m[P + "B2_lnb"] = bc128(inp["ln2_b"][l])
        m[P + "B2_swg"] = np.ascontiguousarray(inp["sh_w_gate"][l]); m[P + "B2_swu"] = np.ascontiguousarray(inp["sh_w_up"][l])
        m[P + "B2_swd"] = np.ascontiguousarray(inp["sh_w_down"][l])
        m[P + "wg_all"] = np.asarray(inp["exp_w_gate"][l]).reshape(64 * 1024, 256)
        m[P + "wu_all"] = np.asarray(inp["exp_w_up"][l]).reshape(64 * 1024, 256)
        m[P + "wd_all"] = np.asarray(inp["exp_w_down"][l]).reshape(64 * 256, 1024)
    return m


def kernel_mega(inp, nl=4, loff=0):
    key = f"MEGA{nl}"
    if key not in _PROGS:
        p = build_MEGA(nl)
        p.S.finish()
        _PROGS[key] = p
    p = _PROGS[key]
    CONST = make_consts()
    maps = [mega_inputs(inp, r, CONST, nl, loff) for r in range(8)]
    res = run_bass_kernel_spmd(p.nc, maps, core_ids=list(range(8))).results
    del maps
    out = np.zeros((4, 4096, 1024), np.float32)
    outc = np.zeros((4, 256, 1024), np.float32)
    for r in range(8):
        b, hf = r // 2, r % 2
        y = np.asarray(res[r]["y"])
        out[b, hf * 2048:(hf + 1) * 2048] = y[0:2048]
        outc[b, hf * 128:(hf + 1) * 128] = y[2048:2176]
    return out, outc


def kernel(**inp):
    cur = dict(inp)
    cur["x"] = np.asarray(inp["x"], np.float32)
    cur["ctx"] = np.asarray(inp["ctx"], np.float32)
    for l in range(4):
        x, xc = kernel_mega(cur, nl=1, loff=l)
        cur["x"], cur["ctx"] = x, xc
    return cur["x"]
```
